# Optimizing a Trainium2 kernel written in Bass

```python
import jax, jax.numpy as jnp
from jax import lax
import numpy as np

D_MODEL = 1024
BATCH = 2
SEQ = 16384
DEPTH = 2

GRID_W = 64
CTX_LEN = 256
POOL_WINDOWS = (2, 4, 8, 16)
N_POOL_GROUPS = 4
POOL_GROUP_W = D_MODEL // 8
POOL_W = N_POOL_GROUPS * POOL_GROUP_W
HEAD_DIM = 64
N_HEADS = D_MODEL // (2 * HEAD_DIM)
N_KV_HEADS = N_HEADS // 4
Q_REP = N_HEADS // N_KV_HEADS
ATTN_W = N_HEADS * HEAD_DIM
KV_W = N_KV_HEADS * HEAD_DIM
WINDOW = 128
BLOCK = 128
ROPE_BASE = 10000.0
N_BRANCHES = 2
COL_POOL = 0
COL_Q = COL_POOL + POOL_W
COL_K = COL_Q + ATTN_W
COL_V = COL_K + KV_W
COL_GATE = COL_V + KV_W
IN_COLS = COL_GATE + N_BRANCHES * D_MODEL
N_EXPERTS = 32
N_EXPERT_GROUPS = 8
EXPERTS_PER_GROUP = N_EXPERTS // N_EXPERT_GROUPS
TOP_K = 2
D_EXPERT = D_MODEL
EXPERT_BLOCK = 256
DEEPNORM_ALPHA = (2 * DEPTH) ** 0.25
DEEPNORM_BETA = (8 * DEPTH) ** -0.25
LN_EPS = 1e-6

kernel_name = 'hybrid_pool_swa_moe_dit_block'


def layer_norm(x, gain=None, bias=None):
    xf = x.astype(jnp.float32)
    xc = xf - xf.mean(-1, keepdims=True)
    y = xc * lax.rsqrt((xc * xc).mean(-1, keepdims=True) + LN_EPS)
    if gain is not None:
        y = y * gain.astype(jnp.float32) + bias.astype(jnp.float32)
    return y.astype(x.dtype)


def modulate(x, shift, scale):
    return layer_norm(x) * (1 + scale) + shift


def ada_terms(cond, w_ada, b_ada):
    return jnp.split(jax.nn.silu(cond) @ w_ada + b_ada, 6, axis=-1)


def axial_rope_tables(rows):
    row = jnp.repeat(jnp.arange(rows), GRID_W).astype(jnp.float32)
    col = jnp.tile(jnp.arange(GRID_W), rows).astype(jnp.float32)
    half = HEAD_DIM // 2
    inv_freq = ROPE_BASE ** (-jnp.arange(0, half, 2, dtype=jnp.float32) / half)
    ang_r = row[:, None] * inv_freq
    ang_c = col[:, None] * inv_freq
    return (jnp.cos(ang_r), jnp.sin(ang_r), jnp.cos(ang_c), jnp.sin(ang_c))


def rotate(x, cos, sin):
    x1, x2 = jnp.split(x, 2, axis=-1)
    c = cos[:, None, :].astype(x.dtype)
    s = sin[:, None, :].astype(x.dtype)
    return jnp.concatenate([x1 * c - x2 * s, x2 * c + x1 * s], axis=-1)


def apply_axial_rope(x, tables):
    cr, sr, cc, sc = tables
    x_row, x_col = jnp.split(x, 2, axis=-1)
    return jnp.concatenate([rotate(x_row, cr, sr), rotate(x_col, cc, sc)], axis=-1)


def sink_softmax(s, sink):
    m = jnp.maximum(jnp.max(s, axis=-1, keepdims=True), sink)
    e = jnp.exp(s - m)
    return e / (jnp.sum(e, axis=-1, keepdims=True) + jnp.exp(sink - m))


def multiscale_pool(u, w_grp, scale):
    B, L, _ = u.shape
    cs = jnp.pad(jnp.cumsum(u.astype(jnp.float32), axis=1), ((0, 0), (1, 0), (0, 0)))
    t = jnp.arange(L)
    means = []
    for g, w in enumerate(POOL_WINDOWS):
        lo = jnp.clip(t - w // 2, 0, L - 1)
        hi = jnp.clip(t - w // 2 + w - 1, 0, L - 1)
        csg = cs[:, :, g * POOL_GROUP_W:(g + 1) * POOL_GROUP_W]
        cnt = (hi - lo + 1).astype(jnp.float32)[None, :, None]
        means.append((jnp.take(csg, hi + 1, axis=1) - jnp.take(csg, lo, axis=1)) / cnt)
    pooled = jnp.concatenate(means, axis=-1).astype(u.dtype) - u
    y = jnp.einsum('blgc,gcd->blgd', pooled.reshape(B, L, N_POOL_GROUPS, POOL_GROUP_W), w_grp)
    return y.reshape(B, L, POOL_W) * scale


def windowed_gqa(q, k, v, kc, vc, sink):
    B, L = q.shape[:2]
    nb = L // BLOCK
    qb = q.reshape(B, nb, BLOCK, N_KV_HEADS, Q_REP, HEAD_DIM)

    def band(t):
        tp = jnp.pad(t, ((0, 0), (BLOCK, BLOCK), (0, 0), (0, 0))).reshape(B, nb + 2, BLOCK, N_KV_HEADS, HEAD_DIM)
        return jnp.concatenate([tp[:, :-2], tp[:, 1:-1], tp[:, 2:]], axis=2)

    kb, vb = band(k), band(v)
    scale = HEAD_DIM ** -0.5
    s_loc = jnp.einsum('bnqgrd,bnkgd->bngrqk', qb, kb).astype(jnp.float32) * scale
    s_ctx = jnp.einsum('bnqgrd,bcgd->bngrqc', qb, kc).astype(jnp.float32) * scale
    blk = jnp.arange(nb)[:, None, None] * BLOCK
    qpos = blk + jnp.arange(BLOCK)[None, :, None]
    kpos = blk - BLOCK + jnp.arange(3 * BLOCK)[None, None, :]
    valid = (jnp.abs(qpos - kpos) <= WINDOW) & (kpos >= 0) & (kpos < L)
    s_loc = jnp.where(valid[None, :, None, None], s_loc, -jnp.inf)
    sk = sink.astype(jnp.float32).reshape(N_KV_HEADS, Q_REP, 1, 1)
    p = sink_softmax(jnp.concatenate([s_loc, s_ctx], axis=-1), sk).astype(v.dtype)
    nk = 3 * BLOCK
    o = (jnp.einsum('bngrqk,bnkgd->bnqgrd', p[..., :nk], vb)
         + jnp.einsum('bngrqc,bcgd->bnqgrd', p[..., nk:], vc))
    return o.reshape(B, L, ATTN_W)


def context_gqa(qc, kc, vc, sink):
    B, C = qc.shape[:2]
    qg = qc.reshape(B, C, N_KV_HEADS, Q_REP, HEAD_DIM)
    s = jnp.einsum('bqgrd,bkgd->bgrqk', qg, kc).astype(jnp.float32) * HEAD_DIM ** -0.5
    sk = sink.astype(jnp.float32).reshape(N_KV_HEADS, Q_REP, 1, 1)
    p = sink_softmax(s, sk).astype(vc.dtype)
    return jnp.einsum('bgrqk,bkgd->bqgrd', p, vc).reshape(B, C, ATTN_W)


def merge_branches(gate_cols, pool_out, attn_out, w_pool_br, w_attn_br, w_o):
    g_pool, g_attn = jnp.split(jax.nn.sigmoid(gate_cols), N_BRANCHES, axis=-1)
    return (g_pool * (pool_out @ w_pool_br) + g_attn * (attn_out @ w_attn_br)) @ w_o


def token_mixer(h, hc, rope, w_in, w_pool_grp, pool_scale, w_pool_br, w_attn_br, sink, w_o, ctx_out):
    B, L, _ = h.shape
    C = hc.shape[1]
    p = h @ w_in
    q = apply_axial_rope(p[..., COL_Q:COL_K].reshape(B, L, N_HEADS, HEAD_DIM), rope)
    k = apply_axial_rope(p[..., COL_K:COL_V].reshape(B, L, N_KV_HEADS, HEAD_DIM), rope)
    v = p[..., COL_V:COL_GATE].reshape(B, L, N_KV_HEADS, HEAD_DIM)
    if ctx_out:
        pc = hc @ w_in
        kvc = pc[..., COL_K:COL_GATE]
    else:
        kvc = hc @ w_in[:, COL_K:COL_GATE]
    kc = kvc[..., :KV_W].reshape(B, C, N_KV_HEADS, HEAD_DIM)
    vc = kvc[..., KV_W:].reshape(B, C, N_KV_HEADS, HEAD_DIM)
    pool_lat = multiscale_pool(p[..., COL_POOL:COL_Q], w_pool_grp, pool_scale)
    attn_lat = windowed_gqa(q, k, v, kc, vc, sink)
    y = merge_branches(p[..., COL_GATE:], pool_lat, attn_lat, w_pool_br, w_attn_br, w_o)
    if not ctx_out:
        return y, None
    pool_c = multiscale_pool(pc[..., COL_POOL:COL_Q], w_pool_grp, pool_scale)
    attn_c = context_gqa(pc[..., COL_Q:COL_K].reshape(B, C, N_HEADS, HEAD_DIM), kc, vc, sink)
    yc = merge_branches(pc[..., COL_GATE:], pool_c, attn_c, w_pool_br, w_attn_br, w_o)
    return y, yc


def route(h, w_router, router_bias):
    N = h.shape[0]
    scores = jax.nn.sigmoid((h @ w_router).astype(jnp.float32))
    biased = (scores + router_bias.astype(jnp.float32)).reshape(N, N_EXPERT_GROUPS, EXPERTS_PER_GROUP)
    group_score = lax.top_k(biased, 2)[0].sum(-1)
    g_sel = jnp.argmax(group_score, axis=-1).astype(jnp.int32)
    in_group = jnp.take_along_axis(biased, g_sel[:, None, None], axis=1)[:, 0]
    _, local = lax.top_k(in_group, TOP_K)
    experts = (g_sel[:, None] * EXPERTS_PER_GROUP + local).astype(jnp.int32)
    w = jnp.take_along_axis(scores, experts, axis=1)
    return experts, (w / w.sum(-1, keepdims=True)).astype(h.dtype)


def expert_ffn(h, experts, weights, w_gate, w_up, w_down):
    N, D = h.shape
    M = N * TOP_K
    flat_e = experts.reshape(M)
    flat_tok = jnp.repeat(jnp.arange(N, dtype=jnp.int32), TOP_K)
    order = jnp.argsort(flat_e)
    e_sorted = flat_e[order]
    counts = jnp.bincount(flat_e, length=N_EXPERTS)
    padded = (counts + EXPERT_BLOCK - 1) // EXPERT_BLOCK * EXPERT_BLOCK
    starts = jnp.cumsum(counts) - counts
    pends = jnp.cumsum(padded)
    pstarts = pends - padded
    dest = (pstarts[e_sorted] + jnp.arange(M) - starts[e_sorted]).astype(jnp.int32)
    n_rows = -(-M // EXPERT_BLOCK) * EXPERT_BLOCK + N_EXPERTS * EXPERT_BLOCK
    n_blocks = n_rows // EXPERT_BLOCK
    row_tok = jnp.full((n_rows,), N, jnp.int32).at[dest].set(flat_tok[order])
    block_expert = jnp.minimum(
        jnp.searchsorted(pends, jnp.arange(n_blocks) * EXPERT_BLOCK, side='right'), N_EXPERTS - 1).astype(jnp.int32)
    h_pad = jnp.concatenate([h, jnp.zeros((1, D), h.dtype)], axis=0)
    xb = h_pad[row_tok].reshape(n_blocks, EXPERT_BLOCK, D)

    def run_block(args):
        xblk, e = args
        return (jax.nn.silu(xblk @ w_gate[e]) * (xblk @ w_up[e])) @ w_down[e]

    yb = lax.map(run_block, (xb, block_expert)).reshape(n_rows, D)
    slot = jnp.zeros((M,), jnp.int32).at[order].set(dest)
    return jnp.einsum('nkd,nk->nd', yb[slot].reshape(N, TOP_K, D), weights)


def setup_inputs(seed: int = 0) -> dict:
    key = jax.random.key(seed)
    ks = jax.random.split(key, 24)

    def nrm(k, shape, s):
        return jax.random.normal(k, shape, jnp.float32) * s

    D = D_MODEL
    w_in = nrm(ks[4], (DEPTH, D, IN_COLS), D ** -0.5)
    w_in = w_in.at[:, :, COL_V:COL_GATE].multiply(DEEPNORM_BETA)
    return {
        'x': nrm(ks[0], (BATCH, SEQ, D), 1.0),
        'c': nrm(ks[1], (BATCH, D), 1.0),
        'ctx': nrm(ks[2], (BATCH, CTX_LEN, D), 1.0),
        'c_ctx': nrm(ks[3], (D,), 1.0),
        'w_ada': nrm(ks[5], (DEPTH, D, 6 * D), 0.5 * D ** -0.5),
        'b_ada': nrm(ks[6], (DEPTH, 6 * D), 0.02),
        'w_in': w_in,
        'w_pool_grp': nrm(ks[7], (DEPTH, N_POOL_GROUPS, POOL_GROUP_W, POOL_GROUP_W), POOL_GROUP_W ** -0.5),
        'pool_scale': 1.0 + nrm(ks[8], (DEPTH, POOL_W), 0.1),
        'w_pool_br': nrm(ks[9], (DEPTH, POOL_W, D), DEEPNORM_BETA * POOL_W ** -0.5),
        'w_attn_br': nrm(ks[10], (DEPTH, ATTN_W, D), DEEPNORM_BETA * ATTN_W ** -0.5),
        'attn_sink': nrm(ks[11], (DEPTH, N_HEADS), 0.5),
        'w_o': nrm(ks[12], (DEPTH, D, D), DEEPNORM_BETA * D ** -0.5),
        'ln1_g': 1.0 + nrm(ks[13], (DEPTH, D), 0.02),
        'ln1_b': nrm(ks[14], (DEPTH, D), 0.02),
        'w_router': nrm(ks[15], (D, N_EXPERTS), D ** -0.5),
        'router_bias': nrm(ks[16], (N_EXPERTS,), 0.01),
        'w_exp_gate': nrm(ks[17], (DEPTH, N_EXPERTS, D, D_EXPERT), D ** -0.5),
        'w_exp_up': nrm(ks[18], (DEPTH, N_EXPERTS, D, D_EXPERT), D ** -0.5),
        'w_exp_down': nrm(ks[19], (DEPTH, N_EXPERTS, D_EXPERT, D), DEEPNORM_BETA * D_EXPERT ** -0.5),
        'ln2_g': 1.0 + nrm(ks[20], (DEPTH, D), 0.02),
        'ln2_b': nrm(ks[21], (DEPTH, D), 0.02),
    }


def reference(x, c, ctx, c_ctx, w_ada, b_ada, w_in, w_pool_grp, pool_scale, w_pool_br, w_attn_br,
              attn_sink, w_o, ln1_g, ln1_b, w_router, router_bias, w_exp_gate, w_exp_up, w_exp_down,
              ln2_g, ln2_b):
    B, L, D = x.shape
    C = ctx.shape[1]
    ROWS = L // GRID_W
    rope = axial_rope_tables(ROWS)
    xc = ctx
    for l in range(DEPTH):
        ctx_out = l < DEPTH - 1
        sh1, sc1, g1, sh2, sc2, g2 = ada_terms(c[:, None, :], w_ada[l], b_ada[l])
        csh1, csc1, cg1, csh2, csc2, cg2 = ada_terms(c_ctx, w_ada[l], b_ada[l])
        h = modulate(x, sh1, sc1)
        hc = modulate(xc, csh1, csc1)
        y, yc = token_mixer(h, hc, rope, w_in[l], w_pool_grp[l], pool_scale[l], w_pool_br[l],
                            w_attn_br[l], attn_sink[l], w_o[l], ctx_out)
        x = layer_norm(DEEPNORM_ALPHA * x + g1 * y, ln1_g[l], ln1_b[l])
        h = modulate(x, sh2, sc2)
        if ctx_out:
            xc = layer_norm(DEEPNORM_ALPHA * xc + cg1 * yc, ln1_g[l], ln1_b[l])
            hc = modulate(xc, csh2, csc2)
            tokens = jnp.concatenate([h.reshape(B * L, D), hc.reshape(B * C, D)], axis=0)
        else:
            tokens = h.reshape(B * L, D)
        experts, weights = route(tokens, w_router, router_bias)
        f = expert_ffn(tokens, experts, weights, w_exp_gate[l], w_exp_up[l], w_exp_down[l])
        x = layer_norm(DEEPNORM_ALPHA * x + g2 * f[:B * L].reshape(B, L, D), ln2_g[l], ln2_b[l])
        if ctx_out:
            xc = layer_norm(DEEPNORM_ALPHA * xc + cg2 * f[B * L:].reshape(B, C, D), ln2_g[l], ln2_b[l])
    return x
```

```python
from contextlib import ExitStack
import numpy as np
import concourse.bass as bass
import concourse.mybir as mybir
from concourse.bass_utils import run_bass_kernel_spmd

F32 = mybir.dt.float32
BF16 = mybir.dt.bfloat16
I32 = mybir.dt.int32
U32 = mybir.dt.uint32
AF = mybir.ActivationFunctionType
ALU = mybir.AluOpType
AX = mybir.AxisListType

D = 1024
DEPTH = 2
L_SEQ = 16384
CTX = 256
NCORE = 8
OWN = 4096
HALO = 256
NTOK = OWN + 2 * HALO
NT = NTOK // 128
NE = 32
SLOT_CAPS = [768] * 4 + [640] * 4 + [512] * 8 + [384] * 12 + [256] * 4
SLOT_OFF = [int(sum(SLOT_CAPS[:i])) for i in range(NE)]
NSLOT = int(sum(SLOT_CAPS))
ALPHA = float((2 * DEPTH) ** 0.25)
EPS = 1e-6
UPAD = 8
NDBG = 0


class _E:
    def __init__(self, name, eng, sem):
        self.name, self.eng, self.sem, self.cnt, self.known = name, eng, sem, 0, {}


class Sched:
    def __init__(self, nc, ndma=20):
        self.nc = nc
        self.E = {}
        for name, eng in (("pe", nc.tensor), ("act", nc.scalar), ("dve", nc.vector),
                          ("pool", nc.gpsimd), ("sp", nc.sync)):
            self.E[name] = _E(name, eng, nc.alloc_semaphore("c_" + name))
        self.dsem = [nc.alloc_semaphore("d%d" % i) for i in range(ndma)]
        self.dval = [0] * ndma
        self.dnext = 0
        self.last_w = {}
        self.readers = {}
        self.nops = 0
        self.rec = None

    def play(self, la, lb=()):
        assert self.rec is None
        ia = ib = 0
        na, nb_ = len(la), len(lb)
        while ia < na or ib < nb_:
            if ib >= nb_ or (ia < na and ia * max(nb_, 1) <= ib * max(na, 1)):
                it = la[ia]
                ia += 1
            else:
                it = lb[ib]
                ib += 1
            kind, eng, fn, rd, wr = it
            (self.op if kind == "op" else self.dma)(eng, fn, rd, wr)

    def _wait(self, e, tok):
        if tok[0] == "e":
            _, name, n = tok
            if name == e.name and name == "pe":
                return
            key = ("e", name)
            if e.known.get(key, 0) >= n:
                return
            e.eng.wait_ge(self.E[name].sem, n)
            e.known[key] = n
        else:
            _, si, v = tok
            key = ("d", si)
            if e.known.get(key, 0) >= v:
                return
            e.eng.wait_ge(self.dsem[si], v)
            e.known[key] = v

    def _deps(self, e, reads, writes):
        deps = []
        for r in reads:
            t = self.last_w.get(r)
            if t is not None:
                deps.append(t)
        for w in writes:
            t = self.last_w.get(w)
            if t is not None:
                deps.append(t)
            deps.extend(self.readers.get(w, ()))
        for t in deps:
            self._wait(e, t)

    def _commit(self, tok, reads, writes):
        for r in reads:
            lst = self.readers.setdefault(r, [])
            if tok[0] == "e":
                lst[:] = [x for x in lst if not (x[0] == "e" and x[1] == tok[1])]
            lst.append(tok)
        for w in writes:
            self.last_w[w] = tok
            self.readers[w] = []

    def op(self, engname, fn, reads=(), writes=()):
        if self.rec is not None:
            self.rec.append(("op", engname, fn, tuple(reads), tuple(writes)))
            return
        e = self.E[engname]
        self._deps(e, reads, writes)
        inst = fn(e.eng)
        e.cnt += 1
        inst.then_inc(e.sem, 1)
        self.nops += 1
        self._commit(("e", engname, e.cnt), reads, writes)

    def dma(self, qname, fn, reads=(), writes=()):
        if self.rec is not None:
            self.rec.append(("dma", qname, fn, tuple(reads), tuple(writes)))
            return
        e = self.E[qname]
        self._deps(e, reads, writes)
        si = self.dnext
        self.dnext = (self.dnext + 1) % len(self.dsem)
        if self.dval[si] > 0:
            self._wait(e, ("d", si, self.dval[si]))
        inst = fn(e.eng)
        self.dval[si] += 16
        inst.then_inc(self.dsem[si], 16)
        self.nops += 1
        self._commit(("d", si, self.dval[si]), reads, writes)

    def barrier(self):
        toks = [("e", n, x.cnt) for n, x in self.E.items() if x.cnt > 0]
        toks += [("d", i, v) for i, v in enumerate(self.dval) if v > 0]
        for e in self.E.values():
            for t in toks:
                if t[0] == "e" and t[1] == e.name:
                    continue
                self._wait(e, t)
        self.last_w.clear()
        self.readers.clear()


class _Stop(Exception):
    pass


def build_program(stop=None):
    nc = bass.Bass("TRN2", target_bir_lowering=False)
    dumps = []

    def dump(name, src, shape, dt=F32, rd=()):
        t = nc.dram_tensor("dbg_" + name, list(shape), dt, kind="ExternalOutput").ap()
        dumps.append((t, src, rd, name))

    def finish(tag):
        if stop != tag:
            return
        S.barrier()
        for (t, src, rd, name) in dumps:
            dma("sp", lambda q, t=t, src=src: q.dma_start(out=t, in_=src), reads=list(rd), writes=["dbg_" + name])
        S.barrier()
        raise _Stop()

    def din(name, shape, dt=F32):
        return nc.dram_tensor(name, list(shape), dt, kind="ExternalInput").ap()

    def dscr(name, shape, dt):
        return nc.dram_tensor(name, list(shape), dt, kind="Internal").ap()

    xe = din("xe", [NTOK, D])
    ctxin = din("ctxin", [CTX, D])
    cT2 = din("cT2", [128, 8, 2])
    cTrep = din("cTrep", [2, 128, 8, 128])
    w_ada = din("w_ada", [DEPTH, D, 6 * D])
    bada_fm = din("bada_fm", [DEPTH, 128, 16])
    bada_rep = din("bada_rep", [DEPTH, 128, 4 * D])
    w_in = din("w_in", [DEPTH, D, 3328])
    w_pg = din("w_pg", [DEPTH, 4, 128, 128])
    pscale = din("pscale", [DEPTH, 128, 4])
    w_pbr = din("w_pbr", [DEPTH, 512, D])
    w_abr = din("w_abr", [DEPTH, 512, D])
    sink_rep = din("sink_rep", [DEPTH, 128, 8])
    w_o = din("w_o", [DEPTH, D, D])
    ln_rep = din("ln_rep", [DEPTH, 4, 128, D])
    w_router = din("w_router", [D, NE])
    rbias_rep = din("rbias_rep", [128, NE])
    w_eg = din("w_eg", [DEPTH * NE * 128, 8 * D])
    w_eu = din("w_eu", [DEPTH * NE * 128, 8 * D])
    w_ed = din("w_ed", [DEPTH * NE * 128, 8 * D])
    ropeC = din("ropeC", [128, NTOK])
    ropeS = din("ropeS", [128, NTOK])
    rotm = din("rotm", [128, 128])
    ident_in = din("ident", [128, 128])
    mtri = din("mtri", [2, 128, 128])
    ltm = din("ltm", [2, 128, 128])
    vmask = din("vmask", [128, NT + 2])
    pinv = din("pinv", [4, 128, 4, 128])
    eoff = din("eoff", [128, NE])
    slottab = din("slottab", [128, 2, NE])
    tri32 = din("tri32", [128, NE, NE])
    pk = din("pk", [128, 9])
    dumpp = din("dumpp", [128, 1])
    out = nc.dram_tensor("out", [OWN, D], F32, kind="ExternalOutput").ap()

    NST = NT + 2
    xbufA = dscr("xbufA", [NST * 128, D], F32)
    xbufB = dscr("xbufB", [NST * 128, D], F32)
    UW = UPAD + NTOK + UPAD
    ubuf = dscr("ubuf", [512, UW], BF16)
    UWC = UPAD + CTX + UPAD
    ubufc = dscr("ubufc", [512, UWC], BF16)
    xg = dscr("xg", [NSLOT + 128, D], BF16)
    yg = dscr("yg", [NSLOT + 128, D], F32)
    g2buf = dscr("g2buf", [DEPTH, 128, 2, D], F32)
    sdram = dscr("sdram", [128, 36, NE], F32)
    hbuf = dscr("hbuf", [36 * 128, D], BF16)

    S = Sched(nc)
    op, dma = S.op, S.dma

    uid = [0]

    def sb(stack, name, shape, dt):
        uid[0] += 1
        return stack.enter_context(nc.sbuf_tensor("%s_%d" % (name, uid[0]), list(shape), dt))

    top = ExitStack()
    PS = [top.enter_context(nc.psum_tensor("psb%d" % i, [128, 512], F32)) for i in range(8)]

    ident_f = sb(top, "ident_f", [128, 128], F32)
    ident_b = sb(top, "ident_b", [128, 128], BF16)
    rot_b = sb(top, "rot_b", [128, 128], BF16)
    mtri_b = sb(top, "mtri_b", [128, 2, 128], BF16)
    lt_b = sb(top, "lt_b", [128, 2, 128], BF16)
    vm = sb(top, "vm", [128, NT + 2], F32)
    eoff_s = sb(top, "eoff_s", [128, NE], F32)
    dumpp_s = sb(top, "dumpp_s", [128, 1], F32)
    rbias_s = sb(top, "rbias_s", [128, NE], F32)
    wr_s = sb(top, "wr_s", [128, 8, NE], F32)
    zero_b = sb(top, "zero_b", [128, 16], BF16)
    zero_f = sb(top, "zero_f", [128, 64], F32)
    epsc = sb(top, "epsc", [128, 1], F32)
    dest_i = sb(top, "dest_i", [128, 36, 2], I32)
    wts = sb(top, "wts", [128, 36, 2], F32)
    widx = sb(top, "widx", [128, NE, 8], I32)
    pk_s = sb(top, "pk_s", [128, 9], F32)
    slot_s = sb(top, "slot_s", [128, 2, NE], F32)

    dma("sp", lambda q: q.dma_start(out=ident_f[:], in_=ident_in), writes=["ident_f"])
    dma("pool", lambda q: q.dma_start(out=ident_b[:], in_=ident_in), writes=["ident_b"])
    dma("pool", lambda q: q.dma_start(out=rot_b[:], in_=rotm), writes=["rot_b"])
    dma("pool", lambda q: q.dma_start(out=mtri_b[:], in_=mtri.rearrange("a p n -> p a n")), writes=["mtri_b"])
    dma("pool", lambda q: q.dma_start(out=lt_b[:], in_=ltm.rearrange("a p n -> p a n")), writes=["lt_b"])
    dma("sp", lambda q: q.dma_start(out=vm[:], in_=vmask), writes=["vm"])
    dma("sp", lambda q: q.dma_start(out=eoff_s[:], in_=eoff), writes=["eoff_s"])
    dma("sp", lambda q: q.dma_start(out=dumpp_s[:], in_=dumpp), writes=["dumpp_s"])
    dma("sp", lambda q: q.dma_start(out=pk_s[:], in_=pk), writes=["pk_s"])
    dma("sp", lambda q: q.dma_start(out=slot_s[:], in_=slottab), writes=["slot_s"])
    dma("sp", lambda q: q.dma_start(out=rbias_s[:], in_=rbias_rep), writes=["rbias_s"])
    dma("sp", lambda q: q.dma_start(out=wr_s[:], in_=w_router.rearrange("(kc p) n -> p kc n", p=128)), writes=["wr_s"])
    op("dve", lambda v: v.memset(zero_b[:], 0.0), writes=["zero_b"])
    op("dve", lambda v: v.memset(zero_f[:], 0.0), writes=["zero_f"])
    op("dve", lambda v: v.memset(epsc[:], EPS), writes=["epsc"])
    for g in range(4):
        for (buf, w, nm) in ((ubuf, UW, "ubuf"), (ubufc, UWC, "ubufc")):
            dma("sp", lambda q, buf=buf, g=g: q.dma_start(out=buf[g * 128:(g + 1) * 128, 0:UPAD], in_=zero_b[:, 0:UPAD]),
                reads=["zero_b"], writes=[nm + "_padl"])
            dma("sp", lambda q, buf=buf, g=g, w=w: q.dma_start(out=buf[g * 128:(g + 1) * 128, w - UPAD:w], in_=zero_b[:, 0:UPAD]),
                reads=["zero_b"], writes=[nm + "_padr"])
    for qd in range(16):
        dma("sp", lambda q, qd=qd: q.dma_start(out=yg[NSLOT:NSLOT + 128, qd * 64:(qd + 1) * 64], in_=zero_f[:]),
            reads=["zero_f"], writes=["yg_dump%d" % qd])

    def xsrc(layer, st):
        if layer == 0:
            if st < NT:
                return xe[st * 128:(st + 1) * 128, :]
            return ctxin[(st - NT) * 128:(st - NT + 1) * 128, :]
        return xbufB[st * 128:(st + 1) * 128, :]

    try:
      for layer in range(DEPTH):
        last = layer == DEPTH - 1
        if layer == 0:
            kv_tiles = list(range(NT)) + [NT, NT + 1]
            q_tiles = list(range(1, NT - 1)) + [NT, NT + 1]
        else:
            kv_tiles = list(range(1, NT - 1)) + [NT, NT + 1]
            q_tiles = list(range(2, NT - 2))
        n_moe = len(q_tiles)

        with ExitStack() as lay:
            mod_fm = sb(lay, "mod_fm", [128, 16, 2], F32)
            rep = sb(lay, "rep", [128, 2, 3, D], F32)
            with ExitStack() as ph:
                sc_f = sb(ph, "sc_f", [128, 8, 2], F32)
                sc_rep = sb(ph, "sc_rep", [128, 2, 8, 128], F32)
                bfm = sb(ph, "bfm", [128, 16], F32)
                brep = sb(ph, "brep", [128, 4 * D], F32)
                g2t = sb(ph, "g2t", [128, 2, D], F32)
                wa = [sb(ph, "wa%d" % i, [128, 8, 512], F32) for i in range(2)]
                dma("sp", lambda q: q.dma_start(out=sc_f[:], in_=cT2), writes=["sc_f"])
                dma("sp", lambda q: q.dma_start(out=sc_rep[:], in_=cTrep.rearrange("a p k m -> p a k m")), writes=["sc_rep"])
                dma("sp", lambda q: q.dma_start(out=bfm[:], in_=bada_fm[layer]), writes=["bfm"])
                dma("sp", lambda q: q.dma_start(out=brep[:], in_=bada_rep[layer]), writes=["brep"])
                op("act", lambda a: a.activation(out=sc_f[:], in_=sc_f[:], func=AF.Silu), reads=["sc_f"], writes=["sc_f"])
                op("act", lambda a: a.activation(out=sc_rep[:], in_=sc_rep[:], func=AF.Silu), reads=["sc_rep"], writes=["sc_rep"])
                for blk in range(12):
                    wb = wa[blk % 2]
                    wn = "wa%d" % (blk % 2)
                    dma("sp", lambda q, wb=wb, blk=blk: q.dma_start(
                        out=wb[:], in_=w_ada[layer][:, blk * 512:(blk + 1) * 512].rearrange("(kc p) n -> p kc n", p=128)),
                        writes=[wn])
                    if blk < 4:
                        for j in range(4):
                            fc = blk * 4 + j
                            for kc in range(8):
                                op("pe", lambda t, wb=wb, j=j, kc=kc, fc=fc: t.matmul(
                                    PS[0][:, fc * 2:fc * 2 + 2], wb[:, kc, j * 128:(j + 1) * 128], sc_f[:, kc, :],
                                    start=(kc == 0), stop=(kc == 7)), reads=[wn, "sc_f"], writes=["ps0"])
                    else:
                        ch = (blk - 4) // 2
                        half = (blk - 4) % 2
                        for who in range(2):
                            bank = 1 + who
                            for kc in range(8):
                                op("pe", lambda t, wb=wb, kc=kc, who=who, bank=bank: t.matmul(
                                    PS[bank][:, :], sc_rep[:, who, kc, :], wb[:, kc, :],
                                    start=(kc == 0), stop=(kc == 7)), reads=[wn, "sc_rep"], writes=["ps%d" % bank])
                            dst_ = rep[:, who, ch, half * 512:(half + 1) * 512] if ch < 3 else g2t[:, who, half * 512:(half + 1) * 512]
                            op("dve", lambda v, who=who, bank=bank, ch=ch, half=half, dst_=dst_: v.tensor_tensor(
                                out=dst_, in0=PS[bank][:, :],
                                in1=brep[:, ch * D + half * 512: ch * D + (half + 1) * 512], op=ALU.add),
                                reads=["ps%d" % bank, "brep"], writes=["rep" if ch < 3 else "g2t"])
                for who in range(2):
                    op("dve", lambda v, who=who: v.tensor_tensor(
                        out=mod_fm[:, :, who], in0=PS[0][:, 0:32].rearrange("p (f w) -> p f w", w=2)[:, :, who],
                        in1=bfm[:, :], op=ALU.add), reads=["ps0", "bfm"], writes=["mod_fm"])
                op("dve", lambda v: v.tensor_scalar(mod_fm[:, 8:16, :], mod_fm[:, 8:16, :], 1.0, None, op0=ALU.add),
                   reads=["mod_fm"], writes=["mod_fm"])
                for who in range(2):
                    op("dve", lambda v, who=who: v.tensor_scalar(rep[:, who, 2, :], rep[:, who, 2, :], 1.0, None, op0=ALU.add),
                       reads=["rep"], writes=["rep"])
                dma("sp", lambda q: q.dma_start(out=g2buf[layer], in_=g2t[:]), reads=["g2t"], writes=["g2buf"])
                if stop == "ada":
                    dump("rep", rep[:].rearrange("p a b n -> p (a b n)"), [128, 6 * D])
                    dump("modfm", mod_fm[:].rearrange("p a b -> p (a b)"), [128, 32])
                finish("ada")
                S.barrier()

            with ExitStack() as ph:
                win = sb(ph, "win", [128, 8, 3328], BF16)
                wpg = sb(ph, "wpg", [128, 4, 128], BF16)
                psc = sb(ph, "psc", [128, 4], F32)
                wpb = sb(ph, "wpb", [128, 4, D], BF16)
                wab = sb(ph, "wab", [128, 4, D], BF16)
                wo = sb(ph, "wo", [128, 8, D], BF16)
                esink = sb(ph, "esink", [128, 8], F32)
                lnr = sb(ph, "lnr1", [128, 2, D], F32)
                dma("sp", lambda q: q.dma_start(out=lnr[:], in_=ln_rep[layer][0:2].rearrange("a p n -> p a n")), writes=["lnr"])
                for kc in range(8):
                    dma("pool", lambda q, kc=kc: q.dma_start(out=win[:, kc, :], in_=w_in[layer][kc * 128:(kc + 1) * 128, :]),
                        writes=["win"])
                dma("pool", lambda q: q.dma_start(out=wpg[:], in_=w_pg[layer].rearrange("g c d -> c g d")), writes=["wpg"])
                dma("sp", lambda q: q.dma_start(out=psc[:], in_=pscale[layer]), writes=["psc"])
                dma("pool", lambda q: q.dma_start(out=wpb[:], in_=w_pbr[layer].rearrange("(kc p) n -> p kc n", p=128)), writes=["wpb"])
                dma("pool", lambda q: q.dma_start(out=wab[:], in_=w_abr[layer].rearrange("(kc p) n -> p kc n", p=128)), writes=["wab"])
                dma("pool", lambda q: q.dma_start(out=wo[:], in_=w_o[layer].rearrange("(kc p) n -> p kc n", p=128)), writes=["wo"])
                dma("sp", lambda q: q.dma_start(out=esink[:], in_=sink_rep[layer]), writes=["esink"])
                op("act", lambda a: a.activation(out=esink[:], in_=esink[:], func=AF.Exp), reads=["esink"], writes=["esink"])

                kT = sb(ph, "kT", [128, NTOK + CTX], BF16)
                vA = sb(ph, "vA", [128, NT + 2, 2, 65], BF16)

                xt = [sb(ph, "xt%d" % i, [128, D], F32) for i in range(2)]
                m1 = sb(ph, "m1", [128, D], F32)
                m2 = sb(ph, "m2", [128, D], F32)
                yt = sb(ph, "yt", [128, D], F32)
                hT = sb(ph, "hT", [128, 8, 128], BF16)
                st6 = sb(ph, "st6", [128, 2, 6], F32)
                mv = sb(ph, "mv", [128, 2], F32)
                rstd = sb(ph, "rstd", [128, 1], F32)
                nb = sb(ph, "nb", [128, 1], F32)
                ropc = sb(ph, "ropc", [128, 128], F32)
                rops = sb(ph, "rops", [128, 128], F32)
                qk_sb = sb(ph, "qk_sb", [128, 5, 128], BF16)
                r1 = sb(ph, "r1", [128, 128], F32)
                r2 = sb(ph, "r2", [128, 128], F32)
                uT_t = sb(ph, "uT_t", [128, 4, 128], BF16)

                lnA = dict(st6=st6, mv=mv, rstd=rstd, nb=nb, sfx="")
                lnB = dict(st6=sb(ph, "st6B", [128, 2, 6], F32), mv=sb(ph, "mvB", [128, 2], F32),
                           rstd=sb(ph, "rstdB", [128, 1], F32), nb=sb(ph, "nbB", [128, 1], F32), sfx="B")

                def ln_stats(src, srcname, sc=None):
                    sc = sc or lnA
                    st6, mv, rstd, nb, sfx = sc["st6"], sc["mv"], sc["rstd"], sc["nb"], sc["sfx"]
                    for hh in range(2):
                        op("dve", lambda v, hh=hh: v.bn_stats(out=st6[:, hh, :], in_=src[:, hh * 512:(hh + 1) * 512]),
                           reads=[srcname], writes=["st6" + sfx])
                    op("dve", lambda v: v.bn_aggr(out=mv[:], in_=st6[:].rearrange("p a b -> p (a b)")), reads=["st6" + sfx], writes=["mv" + sfx])
                    op("act", lambda a: a.activation(out=rstd[:], in_=mv[:, 1:2], func=AF.Sqrt, bias=epsc[:], scale=1.0),
                       reads=["mv" + sfx, "epsc"], writes=["rstd" + sfx])
                    op("dve", lambda v: v.reciprocal(out=rstd[:], in_=rstd[:]), reads=["rstd" + sfx], writes=["rstd" + sfx])
                    op("dve", lambda v: v.scalar_tensor_tensor(out=nb[:], in0=mv[:, 0:1], scalar=-1.0, in1=rstd[:],
                                                               op0=ALU.mult, op1=ALU.mult), reads=["mv" + sfx, "rstd" + sfx], writes=["nb" + sfx])

                def make_hT(layer, stile, xbuf, xname, yt_=None, ytn="yt", hT_=None, hTn="hT", sc=None, banks=(0, 1)):
                    yt_ = yt if yt_ is None else yt_
                    hT_ = hT if hT_ is None else hT_
                    sc = sc or lnA
                    who = 0 if stile < NT else 1
                    ln_stats(xbuf, xname, sc)
                    op("act", lambda a: a.activation(out=yt_[:], in_=xbuf[:], func=AF.Identity, bias=sc["nb"][:], scale=sc["rstd"][:]),
                       reads=[xname, "nb" + sc["sfx"], "rstd" + sc["sfx"]], writes=[ytn])
                    for half in range(2):
                        bk = banks[half]
                        for j in range(4):
                            kc = half * 4 + j
                            op("pe", lambda t, kc=kc, j=j, bk=bk: t.transpose(
                                PS[bk][:, j * 128:(j + 1) * 128], yt_[:, kc * 128:(kc + 1) * 128], ident_f[:]),
                                reads=[ytn, "ident_f"], writes=["ps%d" % bk])
                        for j in range(4):
                            kc = half * 4 + j
                            op("act", lambda a, kc=kc, j=j, bk=bk: a.activation(
                                out=hT_[:, kc, :], in_=PS[bk][:, j * 128:(j + 1) * 128], func=AF.Identity,
                                bias=mod_fm[:, kc, who:who + 1], scale=mod_fm[:, 8 + kc, who:who + 1]),
                                reads=["ps%d" % bk, "mod_fm"], writes=[hTn])

                def rope(dst, dstname, nch, tok0):
                    assert nch == 4
                    dma("sp", lambda q: q.dma_start(out=ropc[:], in_=ropeC[:, tok0:tok0 + 128]), writes=["ropc"])
                    dma("sp", lambda q: q.dma_start(out=rops[:], in_=ropeS[:, tok0:tok0 + 128]), writes=["rops"])
                    for c in range(4):
                        op("pe", lambda t, c=c: t.matmul(PS[3][:, c * 128:(c + 1) * 128], rot_b[:], qk_sb[:, c, :], start=True, stop=True),
                           reads=["rot_b", "qk_sb"], writes=["ps3"])
                    cb = ropc[:].unsqueeze(1).to_broadcast([128, 4, 128])
                    sbb = rops[:].unsqueeze(1).to_broadcast([128, 4, 128])
                    rq1 = yt[:, 0:512].rearrange("p (c n) -> p c n", n=128)
                    rq2 = yt[:, 512:1024].rearrange("p (c n) -> p c n", n=128)
                    op("dve", lambda v: v.tensor_tensor(out=rq1, in0=qk_sb[:, 0:4, :], in1=cb, op=ALU.mult),
                       reads=["qk_sb", "ropc"], writes=["yt"])
                    op("dve", lambda v: v.tensor_tensor(out=rq2, in0=PS[3][:, :].rearrange("p (c n) -> p c n", n=128), in1=sbb, op=ALU.mult),
                       reads=["ps3", "rops"], writes=["yt"])
                    op("dve", lambda v: v.tensor_tensor(out=dst, in0=rq1, in1=rq2, op=ALU.add),
                       reads=["yt"], writes=[dstname])

                gates2 = [sb(ph, "gates%d" % i, [128, 2048], BF16) for i in range(2)]
                qT = sb(ph, "qT", [128, 4, 128], BF16)
                uwin = sb(ph, "uwin", [128, 4, 128 + 2 * UPAD], BF16)
                s2 = sb(ph, "s2", [128, 144], F32)
                s4 = sb(ph, "s4", [128, 144], F32)
                s8 = sb(ph, "s8", [128, 144], F32)
                s16 = sb(ph, "s16", [128, 144], F32)
                ptab = sb(ph, "ptab", [128, 4, 128], F32)
                pooled = sb(ph, "pooled", [128, 4, 128], BF16)
                poT2 = [sb(ph, "poT%d" % i, [128, 4, 128], BF16) for i in range(2)]
                PT = sb(ph, "PT", [128, 5, 8, 128], BF16)
                den = sb(ph, "den", [128, 8], F32)
                ao2 = [sb(ph, "ao%d" % i, [128, 512], BF16) for i in range(2)]
                aoT = sb(ph, "aoT", [128, 4, 128], BF16)
                mb = sb(ph, "mb", [128, D], BF16)
                mT = sb(ph, "mT", [128, 8, 128], BF16)
                h2T = sb(ph, "h2T", [128, 8, 128], F32)
                s_t = sb(ph, "s_t", [128, NE], F32)

                def pass1_tile(i, stile):
                    p_ = i % 2
                    xb = xt[p_]
                    xn = "xt%d" % p_
                    isctx = stile >= NT
                    if p_ == 0:
                        yt_, ytn, hT_, hTn, sc, banks, ub_bank, kv_bank = yt, "yt", hT, "hT", lnA, (0, 1), 2, 5
                        uT_, uTn, qch, r1_, r1n, r2_, r2n = uT_t, "uT_t", 4, r1[:], "r1", r2[:], "r2"
                        rc_, rcn, rs_, rsn = ropc[:], "ropc", rops[:], "rops"
                    else:
                        yt_, ytn, hT_, hTn, sc, banks, ub_bank, kv_bank = m1, "m1", mT, "mT", lnB, (3, 7), 4, 6
                        uT_, uTn, qch, r1_, r1n, r2_, r2n = pooled, "pooled", 3, s2[:, 0:128], "s2", s4[:, 0:128], "s4"
                        rc_, rcn, rs_, rsn = s8[:, 0:128], "s8", s16[:, 0:128], "s16"
                    un, kvn, qn = "ps%d" % ub_bank, "ps%d" % kv_bank, "qk_sb%d" % qch
                    dma("sp", lambda q: q.dma_start(out=xb[:], in_=xsrc(layer, stile)), writes=[xn])
                    make_hT(layer, stile, xb, xn, yt_, ytn, hT_, hTn, sc, banks)
                    vms = vm[:, stile:stile + 1]
                    kcol = stile * 128 if not isctx else NTOK + (stile - NT) * 128
                    for g in range(4):
                        for kc in range(8):
                            op("pe", lambda t, g=g, kc=kc: t.matmul(PS[ub_bank][:, g * 128:(g + 1) * 128], win[:, kc, g * 128:(g + 1) * 128],
                                                                   hT_[:, kc, :], start=(kc == 0), stop=(kc == 7)),
                               reads=["win", hTn], writes=[un])
                    for kc in range(8):
                        op("pe", lambda t, kc=kc: t.matmul(PS[kv_bank][:, 0:128], win[:, kc, 1024:1152], hT_[:, kc, :],
                                                           start=(kc == 0), stop=(kc == 7)), reads=["win", hTn], writes=[kvn])
                    for kc in range(8):
                        op("pe", lambda t, kc=kc: t.matmul(PS[kv_bank][:, 128:256], hT_[:, kc, :], win[:, kc, 1152:1280],
                                                           start=(kc == 0), stop=(kc == 7)), reads=["win", hTn], writes=[kvn])
                    op("act", lambda a: a.activation(out=uT_[:].rearrange("p g n -> p (g n)"), in_=PS[ub_bank][:, :],
                                                     func=AF.Copy, scale=vms), reads=[un, "vm"], writes=[uTn])
                    ub, uoff, unm = (ubuf, UPAD + stile * 128, "ubuf") if not isctx else (ubufc, UPAD + (stile - NT) * 128, "ubufc")
                    for g in range(4):
                        dma("sp", lambda q, g=g: q.dma_start(out=ub[g * 128:(g + 1) * 128, uoff:uoff + 128], in_=uT_[:, g, :]),
                            reads=[uTn], writes=[unm + "_%d" % stile])
                    op("act", lambda a: a.activation(
                        out=vA[:, stile, :, 0:64], in_=PS[kv_bank][:, 128:256].rearrange("p (h d) -> p h d", d=64), func=AF.Copy, scale=vms),
                        reads=[kvn, "vm"], writes=["vA_%d" % stile])
                    for hh in range(2):
                        op("dve", lambda v, hh=hh: v.tensor_copy(vA[:, stile, hh, 64:65], vms), reads=["vm"], writes=["vA_%d" % stile])
                    if isctx:
                        op("act", lambda a: a.activation(out=kT[:, kcol:kcol + 128], in_=PS[kv_bank][:, 0:128], func=AF.Copy),
                           reads=[kvn], writes=["kT_%d" % stile])
                    else:
                        op("act", lambda a: a.activation(out=qk_sb[:, qch, :], in_=PS[kv_bank][:, 0:128], func=AF.Copy, scale=vms),
                           reads=[kvn, "vm"], writes=[qn])
                        dma("sp", lambda q: q.dma_start(out=rc_, in_=ropeC[:, kcol:kcol + 128]), writes=[rcn])
                        dma("sp", lambda q: q.dma_start(out=rs_, in_=ropeS[:, kcol:kcol + 128]), writes=[rsn])
                        op("pe", lambda t: t.matmul(PS[kv_bank][:, 256:384], rot_b[:], qk_sb[:, qch, :], start=True, stop=True),
                           reads=["rot_b", qn], writes=[kvn])
                        op("dve", lambda v: v.tensor_tensor(out=r1_, in0=qk_sb[:, qch, :], in1=rc_, op=ALU.mult), reads=[qn, rcn], writes=[r1n])
                        op("dve", lambda v: v.tensor_tensor(out=r2_, in0=PS[kv_bank][:, 256:384], in1=rs_, op=ALU.mult), reads=[kvn, rsn], writes=[r2n])
                        op("dve", lambda v: v.tensor_tensor(out=kT[:, kcol:kcol + 128], in0=r1_, in1=r2_, op=ALU.add),
                           reads=[r1n, r2n], writes=["kT_%d" % stile])

                for i_, stile_ in enumerate(kv_tiles):
                    pass1_tile(i_, stile_)
                if stop == "p1":
                    dump("kT", kT[:], [128, NTOK + CTX], BF16)
                    dump("vA", vA[:].rearrange("p a b c -> p (a b c)"), [128, (NT + 2) * 130], BF16)
                    dump("ubuf", ubuf, [512, UW], BF16)
                    dump("ubufc", ubufc, [512, UWC], BF16)
                finish("p1")
                S.barrier()
                allA, allB = [], []
                def tile_body(mi, stile):
                    isctx = stile >= NT
                    who = 1 if isctx else 0
                    xb = xt[mi % 2]
                    xn = "xt%d" % (mi % 2)
                    gates = gates2[mi % 2]
                    ao = ao2[mi % 2]
                    poT = poT2[mi % 2]
                    gn, aon, pon = "gates%d" % (mi % 2), "ao%d" % (mi % 2), "poT%d" % (mi % 2)
                    recA = []
                    S.rec = recA
                    dma("sp", lambda q, xb=xb, stile=stile: q.dma_start(out=xb[:], in_=xsrc(layer, stile)), writes=[xn])
                    ub, uoff = (ubuf, stile * 128) if not isctx else (ubufc, (stile - NT) * 128)
                    ureads = (["ubuf_%d" % t for t in (stile - 1, stile, stile + 1)] + ["ubuf_padl", "ubuf_padr"]) if not isctx else \
                        ["ubufc_%d" % NT, "ubufc_%d" % (NT + 1), "ubufc_padl", "ubufc_padr"]
                    for g in range(4):
                        dma("sp", lambda q, g=g, ub=ub, uoff=uoff: q.dma_start(out=uwin[:, g, :], in_=ub[g * 128:(g + 1) * 128, uoff:uoff + 144]),
                            reads=ureads, writes=["uwin"])
                    make_hT(layer, stile, xb, xn)
                    for c in range(4):
                        for kc in range(8):
                            op("pe", lambda t, c=c, kc=kc: t.matmul(PS[2][:, c * 128:(c + 1) * 128], win[:, kc, 512 + c * 128:512 + (c + 1) * 128],
                                                                   hT[:, kc, :], start=(kc == 0), stop=(kc == 7)),
                               reads=["win", "hT"], writes=["ps2"])
                    if isctx:
                        op("act", lambda a: a.activation(out=qT[:].rearrange("p c n -> p (c n)"), in_=PS[2][:, :], func=AF.Copy),
                           reads=["ps2"], writes=["qT"])
                    else:
                        op("act", lambda a: a.activation(out=qk_sb[:, 0:4, :].rearrange("p c n -> p (c n)"), in_=PS[2][:, :], func=AF.Copy),
                           reads=["ps2"], writes=["qk_sb"])
                        rope(qT[:], "qT", 4, stile * 128)
                    for blk in range(4):
                        bank = blk % 2
                        for kc in range(8):
                            op("pe", lambda t, blk=blk, kc=kc, bank=bank: t.matmul(
                                PS[bank][:, :], hT[:, kc, :], win[:, kc, 1280 + blk * 512:1280 + (blk + 1) * 512],
                                start=(kc == 0), stop=(kc == 7)), reads=["win", "hT"], writes=["ps%d" % bank])
                        op("act", lambda a, blk=blk, bank=bank: a.activation(out=gates[:, blk * 512:(blk + 1) * 512], in_=PS[bank][:, :],
                                                                             func=AF.Sigmoid), reads=["ps%d" % bank], writes=[gn])
                    edge = None
                    if isctx:
                        edge = 2 + (stile - NT)
                    elif stile == 2:
                        edge = 0
                    elif stile == NT - 3:
                        edge = 1
                    if edge is not None:
                        dma("sp", lambda q, edge=edge: q.dma_start(out=ptab[:], in_=pinv[edge]), writes=["ptab"])
                    for g in range(4):
                        U = uwin[:, g, :]
                        op("dve", lambda v, U=U: v.tensor_tensor(out=s2[:, 1:144], in0=U[:, 0:143], in1=U[:, 1:144], op=ALU.add),
                           reads=["uwin"], writes=["s2"])
                        cur, curname = s2, "s2"
                        if g >= 1:
                            op("dve", lambda v: v.tensor_tensor(out=s4[:, 2:143], in0=s2[:, 1:142], in1=s2[:, 3:144], op=ALU.add),
                               reads=["s2"], writes=["s4"])
                            cur, curname = s4, "s4"
                        if g >= 2:
                            op("dve", lambda v: v.tensor_tensor(out=s8[:, 4:141], in0=s4[:, 2:139], in1=s4[:, 6:143], op=ALU.add),
                               reads=["s4"], writes=["s8"])
                            cur, curname = s8, "s8"
                        if g >= 3:
                            op("dve", lambda v: v.tensor_tensor(out=s16[:, 8:137], in0=s8[:, 4:133], in1=s8[:, 12:141], op=ALU.add),
                               reads=["s8"], writes=["s16"])
                            cur, curname = s16, "s16"
                        wdw = (2, 4, 8, 16)[g]
                        if edge is None:
                            op("dve", lambda v, g=g, cur=cur, U=U, wdw=wdw: v.scalar_tensor_tensor(
                                out=pooled[:, g, :], in0=cur[:, 8:136], scalar=1.0 / wdw, in1=U[:, 8:136], op0=ALU.mult, op1=ALU.subtract),
                                reads=[curname, "uwin"], writes=["pooled"])
                        else:
                            op("dve", lambda v, g=g, cur=cur: v.tensor_tensor(out=r1[:], in0=cur[:, 8:136], in1=ptab[:, g, :], op=ALU.mult),
                               reads=[curname, "ptab"], writes=["r1"])
                            op("dve", lambda v, g=g, U=U: v.tensor_tensor(out=pooled[:, g, :], in0=r1[:], in1=U[:, 8:136], op=ALU.subtract),
                               reads=["r1", "uwin"], writes=["pooled"])
                    for g in range(4):
                        op("pe", lambda t, g=g: t.matmul(PS[2][:, g * 128:(g + 1) * 128], wpg[:, g, :], pooled[:, g, :], start=True, stop=True),
                           reads=["wpg", "pooled"], writes=["ps2"])
                    for g in range(4):
                        op("act", lambda a, g=g: a.activation(out=poT[:, g, :], in_=PS[2][:, g * 128:(g + 1) * 128], func=AF.Copy,
                                                              scale=psc[:, g:g + 1]), reads=["ps2", "psc"], writes=[pon])
                    if isctx:
                        kblocks = [(NTOK, NT, None), (NTOK + 128, NT + 1, None)]
                    else:
                        kblocks = [((stile - 1) * 128, stile - 1, 0), (stile * 128, stile, None), ((stile + 1) * 128, stile + 1, 1),
                                   (NTOK, NT, None), (NTOK + 128, NT + 1, None)]
                    nkb = len(kblocks)
                    for bi, (kc0, vt, msk) in enumerate(kblocks):
                        for ph_ in range(8):
                            c, half = ph_ // 2, ph_ % 2
                            bank = 3 + half
                            op("pe", lambda t, c=c, half=half, bank=bank, ph_=ph_, kc0=kc0: t.matmul(
                                PS[bank][:, c * 128:(c + 1) * 128],
                                kT[half * 64:(half + 1) * 64, kc0:kc0 + 128], qT[half * 64:(half + 1) * 64, c, :],
                                start=True, stop=True), reads=["kT", "qT"], writes=["ps%d" % bank])
                        for hb in range(2):
                            op("act", lambda a, hb=hb, bi=bi: a.activation(
                                out=PT[:, bi, hb * 4:(hb + 1) * 4, :].rearrange("p h n -> p (h n)"), in_=PS[3 + hb][:, :],
                                func=AF.Exp, scale=0.125), reads=["ps%d" % (3 + hb)], writes=["PT"])
                        if msk is not None:
                            for ph_ in range(8):
                                op("pool", lambda v, bi=bi, ph_=ph_, msk=msk: v.tensor_tensor(
                                    out=PT[:, bi, ph_, :], in0=PT[:, bi, ph_, :], in1=mtri_b[:, msk, :], op=ALU.mult),
                                    reads=["PT", "mtri_b"], writes=["PT"])
                    for ph_ in range(8):
                        half = ph_ % 2
                        bank = ph_ // 4
                        for bi, (kc0, vt, msk) in enumerate(kblocks):
                            op("pe", lambda t, ph_=ph_, half=half, bank=bank, bi=bi, vt=vt: t.matmul(
                                PS[bank][:, (ph_ % 4) * 65:(ph_ % 4) * 65 + 65], PT[:, bi, (ph_ % 2) * 4 + ph_ // 2, :], vA[:, vt, half, :],
                                start=(bi == 0), stop=(bi == nkb - 1)), reads=["PT", "vA"], writes=["ps%d" % bank])
                    for hb in range(2):
                        op("dve", lambda v, hb=hb: v.tensor_tensor(
                            out=den[:, hb * 4:(hb + 1) * 4], in0=PS[hb][:, 0:260].rearrange("p (h d) -> p h d", d=65)[:, :, 64],
                            in1=esink[:, hb * 4:(hb + 1) * 4], op=ALU.add), reads=["ps%d" % hb, "esink"], writes=["den"])
                    op("dve", lambda v: v.reciprocal(out=den[:], in_=den[:]), reads=["den"], writes=["den"])
                    for hb in range(2):
                        op("dve", lambda v, hb=hb: v.tensor_tensor(
                            out=ao[:, hb * 256:(hb + 1) * 256].rearrange("p (h d) -> p h d", d=64),
                            in0=PS[hb][:, 0:260].rearrange("p (h d) -> p h d", d=65)[:, :, 0:64],
                            in1=den[:, hb * 4:(hb + 1) * 4].unsqueeze(2).to_broadcast([128, 4, 64]), op=ALU.mult),
                            reads=["ps%d" % hb, "den"], writes=[aon])
                    recB = []
                    S.rec = recB
                    psb = PS[5][:].bitcast(BF16)
                    for c in range(4):
                        op("pe", lambda t, c=c, psb=psb, ao=ao: t.transpose(psb[:, c * 128:(c + 1) * 128], ao[:, c * 128:(c + 1) * 128], ident_b[:]),
                           reads=[aon, "ident_b"], writes=["ps5"])
                    op("act", lambda a, psb=psb: a.activation(out=aoT[:].rearrange("p c n -> p (c n)"), in_=psb[:, 0:512], func=AF.Copy),
                       reads=["ps5"], writes=["aoT"])
                    for half in range(2):
                        for kc in range(4):
                            op("pe", lambda t, half=half, kc=kc: t.matmul(PS[5][:, :], poT[:, kc, :], wpb[:, kc, half * 512:(half + 1) * 512],
                                                                         start=(kc == 0), stop=(kc == 3)), reads=[pon, "wpb"], writes=["ps5"])
                        for kc in range(4):
                            op("pe", lambda t, half=half, kc=kc: t.matmul(PS[6][:, :], aoT[:, kc, :], wab[:, kc, half * 512:(half + 1) * 512],
                                                                         start=(kc == 0), stop=(kc == 3)), reads=["aoT", "wab"], writes=["ps6"])
                        op("dve", lambda v, half=half: v.tensor_tensor(out=m1[:, half * 512:(half + 1) * 512], in0=PS[5][:, :],
                                                                       in1=gates[:, half * 512:(half + 1) * 512], op=ALU.mult),
                           reads=["ps5", gn], writes=["m1"])
                        op("dve", lambda v, half=half: v.tensor_tensor(out=m2[:, half * 512:(half + 1) * 512], in0=PS[6][:, :],
                                                                       in1=gates[:, 1024 + half * 512:1024 + (half + 1) * 512], op=ALU.mult),
                           reads=["ps6", gn], writes=["m2"])
                    op("pool", lambda v: v.tensor_tensor(out=mb[:], in0=m1[:], in1=m2[:], op=ALU.add), reads=["m1", "m2"], writes=["mb"])
                    psb7 = PS[7][:].bitcast(BF16)
                    for kc in range(8):
                        op("pe", lambda t, kc=kc: t.transpose(psb7[:, kc * 128:(kc + 1) * 128], mb[:, kc * 128:(kc + 1) * 128], ident_b[:]),
                           reads=["mb", "ident_b"], writes=["ps7"])
                    op("act", lambda a: a.activation(out=mT[:].rearrange("p c n -> p (c n)"), in_=psb7[:, :], func=AF.Copy),
                       reads=["ps7"], writes=["mT"])
                    for half in range(2):
                        for kc in range(8):
                            op("pe", lambda t, half=half, kc=kc: t.matmul(PS[5 + half][:, :], mT[:, kc, :], wo[:, kc, half * 512:(half + 1) * 512],
                                                                         start=(kc == 0), stop=(kc == 7)), reads=["mT", "wo"], writes=["ps%d" % (5 + half)])
                        op("dve", lambda v, half=half: v.tensor_tensor(out=m1[:, half * 512:(half + 1) * 512], in0=PS[5 + half][:, :],
                                                                       in1=rep[:, who, 0, half * 512:(half + 1) * 512], op=ALU.mult),
                           reads=["ps%d" % (5 + half), "rep"], writes=["m1"])
                    op("dve", lambda v, xb=xb: v.scalar_tensor_tensor(out=m2[:], in0=xb[:], scalar=ALPHA, in1=m1[:], op0=ALU.mult, op1=ALU.add),
                       reads=[xn, "m1"], writes=["m2"])
                    ln_stats(m2, "m2", lnB)
                    op("act", lambda a: a.activation(out=m1[:], in_=m2[:], func=AF.Identity, bias=lnB["nb"][:], scale=lnB["rstd"][:]),
                       reads=["m2", "nbB", "rstdB"], writes=["m1"])
                    op("pool", lambda v: v.tensor_tensor(out=m1[:], in0=m1[:], in1=lnr[:, 0, :], op=ALU.mult), reads=["m1", "lnr"], writes=["m1"])
                    op("pool", lambda v: v.tensor_tensor(out=m1[:], in0=m1[:], in1=lnr[:, 1, :], op=ALU.add), reads=["m1", "lnr"], writes=["m1"])
                    dma("sp", lambda q, stile=stile: q.dma_start(out=xbufA[stile * 128:(stile + 1) * 128, :], in_=m1[:]),
                        reads=["m1"], writes=["xbufA_%d" % stile])
                    ln_stats(m1, "m1", lnB)
                    op("act", lambda a: a.activation(out=m2[:], in_=m1[:], func=AF.Identity, bias=lnB["nb"][:], scale=lnB["rstd"][:]),
                       reads=["m1", "nbB", "rstdB"], writes=["m2"])
                    op("dve", lambda v: v.tensor_tensor(out=m2[:], in0=m2[:], in1=rep[:, who, 2, :], op=ALU.mult), reads=["m2", "rep"], writes=["m2"])
                    op("dve", lambda v: v.tensor_tensor(out=m2[:], in0=m2[:], in1=rep[:, who, 1, :], op=ALU.add), reads=["m2", "rep"], writes=["m2"])
                    op("pool", lambda v: v.tensor_copy(mb[:], m2[:]), reads=["m2"], writes=["mb"])
                    for half in range(2):
                        for j in range(4):
                            kc = half * 4 + j
                            op("pe", lambda t, kc=kc, j=j, half=half: t.transpose(
                                PS[7 - 2 * half][:, j * 128:(j + 1) * 128], m2[:, kc * 128:(kc + 1) * 128], ident_f[:]),
                                reads=["m2", "ident_f"], writes=["ps%d" % (7 - 2 * half)])
                        op("act", lambda a, half=half: a.activation(out=h2T[:, half * 4:(half + 1) * 4, :].rearrange("p c n -> p (c n)"),
                                                                    in_=PS[7 - 2 * half][:, :], func=AF.Copy),
                           reads=["ps%d" % (7 - 2 * half)], writes=["h2T"])
                    for kc in range(8):
                        op("pe", lambda t, kc=kc: t.matmul(PS[6][:, 0:NE], h2T[:, kc, :], wr_s[:, kc, :], start=(kc == 0), stop=(kc == 7)),
                           reads=["h2T", "wr_s"], writes=["ps6"])
                    op("act", lambda a: a.activation(out=s_t[:], in_=PS[6][:, 0:NE], func=AF.Sigmoid), reads=["ps6"], writes=["s_t"])
                    dma("sp", lambda q, mi=mi: q.dma_start(out=sdram[:, mi, :], in_=s_t[:]), reads=["s_t"], writes=["sdram_%d" % mi])
                    dma("sp", lambda q, mi=mi: q.dma_start(out=hbuf[mi * 128:(mi + 1) * 128, :], in_=mb[:]), reads=["mb"], writes=["hbuf_%d" % mi])
                    S.rec = None
                    allA.append(recA)
                    allB.append(recB)

                for mi_, stile_ in enumerate(q_tiles):
                    tile_body(mi_, stile_)
                S.play(allA[0])
                for mi in range(len(allB)):
                    S.play(allA[mi + 1] if mi + 1 < len(allA) else [], allB[mi])
                S.barrier()

            with ExitStack() as ph:
                n = n_moe
                big = {nm: sb(ph, "rb_" + nm, [128, 36, NE], F32) for nm in ("s", "bsd", "msk", "oh1", "oh2", "tmp", "posf", "tq", "tt")}
                G = {nm: sb(ph, "rg_" + nm, [128, 36 * 8], F32) for nm in ("m1", "n1", "m2", "n2", "t1", "t2", "gs", "goh", "pen")}
                Cc = {nm: sb(ph, "rc_" + nm, [128, 36], F32) for nm in ("gmax", "mx1", "mx2", "w1", "w2", "ws", "d", "p", "v", "cs")}
                A_b = sb(ph, "A_b", [128, 36, NE], BF16)
                destf = sb(ph, "destf", [128, 36, 2], F32)
                hb2 = [sb(ph, "hb%d" % i, [128, D], BF16) for i in range(2)]

                def B(nm):
                    return big[nm][:, 0:n, :]

                def G2(nm):
                    return G[nm][:, 0:n * 8]

                def G3(nm):
                    return G[nm][:, 0:n * 8].rearrange("p (n g) -> p n g", g=8)

                def C(nm):
                    return Cc[nm][:, 0:n]

                def bc(ap2, last):
                    return ap2.unsqueeze(2).to_broadcast([128, ap2.shape[1], last])

                def tt(o, a_, b_, o_, rd, wr):
                    op("dve", lambda v: v.tensor_tensor(out=o, in0=a_, in1=b_, op=o_), reads=rd, writes=wr)

                def red(o, a_, o_, rd, wr):
                    op("dve", lambda v: v.tensor_reduce(out=o, in_=a_, axis=AX.X, op=o_), reads=rd, writes=wr)
                dma("sp", lambda q: q.dma_start(out=B("s"), in_=sdram[:, 0:n, :]), writes=["r_s"])
                tt(B("bsd"), B("s"), rbias_s[:].unsqueeze(1).to_broadcast([128, n, NE]), ALU.add, ["r_s", "rbias_s"], ["r_bsd"])
                bv4 = B("bsd").rearrange("p n (g k) -> p (n g) k", k=4)
                tt(G2("m1"), bv4[:, :, 0], bv4[:, :, 1], ALU.max, ["r_bsd"], ["g_m1"])
                tt(G2("n1"), bv4[:, :, 0], bv4[:, :, 1], ALU.min, ["r_bsd"], ["g_n1"])
                tt(G2("m2"), bv4[:, :, 2], bv4[:, :, 3], ALU.max, ["r_bsd"], ["g_m2"])
                tt(G2("n2"), bv4[:, :, 2], bv4[:, :, 3], ALU.min, ["r_bsd"], ["g_n2"])
                tt(G2("t1"), G2("m1"), G2("m2"), ALU.max, ["g_m1", "g_m2"], ["g_t1"])
                tt(G2("t2"), G2("m1"), G2("m2"), ALU.min, ["g_m1", "g_m2"], ["g_t2"])
                tt(G2("n1"), G2("n1"), G2("n2"), ALU.max, ["g_n1", "g_n2"], ["g_n1"])
                tt(G2("t2"), G2("t2"), G2("n1"), ALU.max, ["g_t2", "g_n1"], ["g_t2"])
                tt(G2("gs"), G2("t1"), G2("t2"), ALU.add, ["g_t1", "g_t2"], ["g_gs"])
                red(C("gmax"), G3("gs"), ALU.max, ["g_gs"], ["c_gmax"])
                tt(G3("goh"), G3("gs"), bc(C("gmax"), 8), ALU.is_equal, ["g_gs", "c_gmax"], ["g_goh"])
                op("dve", lambda v: v.tensor_scalar(G2("pen"), G2("goh"), 8.0, -8.0, op0=ALU.mult, op1=ALU.add), reads=["g_goh"], writes=["g_pen"])
                mv4 = B("msk").rearrange("p n (g k) -> p (n g) k", k=4)
                tt(mv4, bv4, bc(G2("goh"), 4), ALU.mult, ["r_bsd", "g_goh"], ["r_msk"])
                tt(mv4, mv4, bc(G2("pen"), 4), ALU.add, ["r_msk", "g_pen"], ["r_msk"])
                red(C("mx1"), B("msk"), ALU.max, ["r_msk"], ["c_mx1"])
                tt(B("oh1"), B("msk"), bc(C("mx1"), NE), ALU.is_equal, ["r_msk", "c_mx1"], ["r_oh1"])
                op("dve", lambda v: v.scalar_tensor_tensor(out=B("tmp"), in0=B("oh1"), scalar=-16.0, in1=B("msk"), op0=ALU.mult, op1=ALU.add),
                   reads=["r_oh1", "r_msk"], writes=["r_tmp"])
                red(C("mx2"), B("tmp"), ALU.max, ["r_tmp"], ["c_mx2"])
                tt(B("oh2"), B("tmp"), bc(C("mx2"), NE), ALU.is_equal, ["r_tmp", "c_mx2"], ["r_oh2"])
                tt(B("tt"), B("oh1"), B("s"), ALU.mult, ["r_oh1", "r_s"], ["r_tt"])
                red(C("w1"), B("tt"), ALU.add, ["r_tt"], ["c_w1"])
                tt(B("tt"), B("oh2"), B("s"), ALU.mult, ["r_oh2", "r_s"], ["r_tt"])
                red(C("w2"), B("tt"), ALU.add, ["r_tt"], ["c_w2"])
                tt(C("ws"), C("w1"), C("w2"), ALU.add, ["c_w1", "c_w2"], ["c_ws"])
                op("dve", lambda v: v.reciprocal(out=C("ws"), in_=C("ws")), reads=["c_ws"], writes=["c_ws"])
                tt(wts[:, 0:n, 0], C("w1"), C("ws"), ALU.mult, ["c_w1", "c_ws"], ["wts"])
                tt(wts[:, 0:n, 1], C("w2"), C("ws"), ALU.mult, ["c_w2", "c_ws"], ["wts"])
                tt(A_b[:, 0:n, :], B("oh1"), B("oh2"), ALU.add, ["r_oh1", "r_oh2"], ["A_b"])
                X1 = sb(ph, "X1", [128, NE, NE], F32)
                X2 = sb(ph, "X2", [128, NE, NE], F32)
                tri_s = sb(ph, "tri_s", [128, NE, NE], F32)
                sm = {nm: sb(ph, "sm_" + nm, [128, NE], F32) for nm in ("cnt", "rank", "eoffd", "capd", "eos", "base")}
                widx_f = sb(ph, "widx_f", [128, NE, 8], F32)
                dma("sp", lambda q: q.dma_start(out=tri_s[:], in_=tri32), writes=["tri_s"])
                for mi in range(n):
                    op("pe", lambda t, mi=mi: t.matmul(PS[3][:, 0:NE], lt_b[:, 1, :], A_b[:, mi, :], start=(mi == 0), stop=(mi == n - 1)),
                       reads=["lt_b", "A_b"], writes=["ps3"])
                op("act", lambda a: a.activation(out=sm["cnt"][:], in_=PS[3][:, 0:NE], func=AF.Copy), reads=["ps3"], writes=["sm_cnt"])
                c_row = sm["cnt"][:].unsqueeze(2).to_broadcast([128, NE, NE])
                c_col = sm["cnt"][:].unsqueeze(1).to_broadcast([128, NE, NE])
                tt(X1[:], c_col, c_row, ALU.is_gt, ["sm_cnt"], ["X1"])
                tt(X2[:], c_col, c_row, ALU.is_equal, ["sm_cnt"], ["X2"])
                tt(X2[:], X2[:], tri_s[:], ALU.mult, ["X2", "tri_s"], ["X2"])
                tt(X1[:], X1[:], X2[:], ALU.add, ["X1", "X2"], ["X1"])
                red(sm["rank"][:], X1[:], ALU.add, ["X1"], ["sm_rank"])
                io_col = eoff_s[:].unsqueeze(1).to_broadcast([128, NE, NE])
                io_row = eoff_s[:].unsqueeze(2).to_broadcast([128, NE, NE])
                tt(X1[:], sm["rank"][:].unsqueeze(2).to_broadcast([128, NE, NE]), io_col, ALU.is_equal, ["sm_rank", "eoff_s"], ["X1"])
                tt(X2[:], X1[:], slot_s[:, 0, :].unsqueeze(1).to_broadcast([128, NE, NE]), ALU.mult, ["X1", "slot_s"], ["X2"])
                red(sm["eoffd"][:], X2[:], ALU.add, ["X2"], ["sm_eoffd"])
                tt(X2[:], X1[:], slot_s[:, 1, :].unsqueeze(1).to_broadcast([128, NE, NE]), ALU.mult, ["X1", "slot_s"], ["X2"])
                red(sm["capd"][:], X2[:], ALU.add, ["X2"], ["sm_capd"])
                tt(X2[:], X1[:], io_row, ALU.mult, ["X1", "eoff_s"], ["X2"])
                red(sm["eos"][:], X2[:].rearrange("p e s -> p s e"), ALU.add, ["X2"], ["sm_eos"])
                op("dve", lambda v: v.tensor_scalar(sm["base"][:], sm["eos"][:], 128.0, pk_s[:, 0:1], op0=ALU.mult, op1=ALU.add),
                   reads=["sm_eos", "pk_s"], writes=["sm_base"])
                op("dve", lambda v: v.tensor_scalar(sm["base"][:], sm["base"][:], float(layer * NE * 128), None, op0=ALU.add),
                   reads=["sm_base"], writes=["sm_base"])
                tt(widx_f[:], sm["base"][:].unsqueeze(2).to_broadcast([128, NE, 8]), pk_s[:, 1:9].unsqueeze(1).to_broadcast([128, NE, 8]),
                   ALU.add, ["sm_base", "pk_s"], ["widx_f"])
                op("dve", lambda v: v.tensor_copy(widx[:], widx_f[:]), reads=["widx_f"], writes=["widx"])
                for mi in range(n):
                    bk = mi // 16
                    reg = PS[bk][:, (mi % 16) * NE:(mi % 16 + 1) * NE]
                    op("pe", lambda t, reg=reg, mi=mi: t.matmul(reg, lt_b[:, 0, :], A_b[:, mi, :], start=True, stop=(mi == 0)),
                       reads=["lt_b", "A_b"], writes=["ps%d" % bk])
                    for j in range(mi):
                        op("pe", lambda t, reg=reg, j=j, mi=mi: t.matmul(reg, lt_b[:, 1, :], A_b[:, j, :], start=False, stop=(j == mi - 1)),
                           reads=["lt_b", "A_b"], writes=["ps%d" % bk])
                for bk in range((n + 15) // 16):
                    t0_, t1_ = bk * 16, min(n, bk * 16 + 16)
                    op("act", lambda a, bk=bk, t0_=t0_, t1_=t1_: a.activation(
                        out=big["posf"][:, t0_:t1_, :], in_=PS[bk][:, 0:(t1_ - t0_) * NE].rearrange("p (n e) -> p n e", e=NE), func=AF.Copy),
                        reads=["ps%d" % bk], writes=["r_posf"])
                tt(B("tq"), B("posf"), sm["eoffd"][:].unsqueeze(1).to_broadcast([128, n, NE]), ALU.add, ["r_posf", "sm_eoffd"], ["r_tq"])
                for k, oh in enumerate(("oh1", "oh2")):
                    tt(B("tt"), B(oh), B("tq"), ALU.mult, ["r_" + oh, "r_tq"], ["r_tt"])
                    red(C("d"), B("tt"), ALU.add, ["r_tt"], ["c_d"])
                    tt(B("tt"), B(oh), B("posf"), ALU.mult, ["r_" + oh, "r_posf"], ["r_tt"])
                    red(C("p"), B("tt"), ALU.add, ["r_tt"], ["c_p"])
                    tt(B("tt"), B(oh), sm["capd"][:].unsqueeze(1).to_broadcast([128, n, NE]), ALU.mult, ["r_" + oh, "sm_capd"], ["r_tt"])
                    red(C("cs"), B("tt"), ALU.add, ["r_tt"], ["c_cs"])
                    tt(C("v"), C("p"), C("cs"), ALU.is_lt, ["c_p", "c_cs"], ["c_v"])
                    op("dve", lambda v: v.tensor_scalar(C("d"), C("d"), dumpp_s[:, 0:1], None, op0=ALU.subtract), reads=["c_d", "dumpp_s"], writes=["c_d"])
                    tt(C("d"), C("d"), C("v"), ALU.mult, ["c_d", "c_v"], ["c_d"])
                    op("dve", lambda v, k=k: v.tensor_scalar(destf[:, 0:n, k], C("d"), dumpp_s[:, 0:1], None, op0=ALU.add),
                       reads=["c_d", "dumpp_s"], writes=["destf"])
                op("dve", lambda v: v.tensor_copy(dest_i[:, 0:n, :], destf[:, 0:n, :]), reads=["destf"], writes=["dest_i"])
                for mi in range(n):
                    hb = hb2[mi % 2]
                    hn = "hb%d" % (mi % 2)
                    dma("sp", lambda q, hb=hb, mi=mi: q.dma_start(out=hb[:], in_=hbuf[mi * 128:(mi + 1) * 128, :]), writes=[hn])
                    for k in range(2):
                        dma("pool", lambda q, mi=mi, k=k, hb=hb: q.indirect_dma_start(
                            out=xg, out_offset=bass.IndirectOffsetOnAxis(ap=dest_i[:, mi, k:k + 1].bitcast(U32), axis=0),
                            in_=hb[:], in_offset=None), reads=[hn, "dest_i"], writes=["xg_%d_%d" % (mi, k)])
                if stop == "p2":
                    dump("xbufA", xbufA, [NST * 128, D])
                    dump("dest", dest_i[:].rearrange("p a b -> p (a b)"), [128, 72], I32)
                    dump("wts", wts[:].rearrange("p a b -> p (a b)"), [128, 72])
                    dump("xg", xg, [NSLOT + 128, D], BF16)
                finish("p2")
                S.barrier()

            with ExitStack() as ph:
                wg = [sb(ph, "wg%d" % i, [128, 8, D], BF16) for i in range(2)]
                wu = [sb(ph, "wu%d" % i, [128, 8, D], BF16) for i in range(2)]
                wd = [sb(ph, "wd%d" % i, [128, 8, D], BF16) for i in range(2)]
                xgt = [sb(ph, "xgt%d" % i, [128, D], BF16) for i in range(2)]
                HC = 384
                XTs = [sb(ph, "XT%d" % i, [128, 8, HC], BF16) for i in range(2)]
                sgs = [sb(ph, "sg%d" % i, [128, HC], BF16) for i in range(2)]
                aTs = [sb(ph, "aT%d" % i, [128, 8, HC], BF16) for i in range(2)]
                yo = [sb(ph, "yo%d" % i, [128, D], F32) for i in range(3)]
                cnt = {"ld": 0, "yi": 0, "sg": 0}
                halves = []
                order = []
                for i_ in range(NE // 2):
                    order += [i_, NE - 1 - i_]
                for pos_, sl in enumerate(order):
                    c_, r_ = SLOT_CAPS[sl], SLOT_OFF[sl]
                    while c_ > 0:
                        w_ = 384 if (c_ >= 384 and c_ != 512) else 256
                        halves.append((pos_, r_, w_))
                        r_ += w_
                        c_ -= w_
                first_of = {}
                for hi_, (sl, r_, w_) in enumerate(halves):
                    first_of.setdefault(sl, hi_)
                wflat = {"wg": w_eg, "wu": w_eu, "wd": w_ed}

                def emit_W(e):
                    par = e % 2
                    for (wbuf, nm) in ((wg[par], "wg"), (wu[par], "wu"), (wd[par], "wd")):
                        dma("pool", lambda q, wbuf=wbuf, nm=nm, e=e: q.indirect_dma_start(
                            out=wbuf[:].rearrange("p k n -> p (k n)"), out_offset=None, in_=wflat[nm],
                            in_offset=bass.IndirectOffsetOnAxis(ap=widx[:, order[e], 0:1].bitcast(U32), axis=0)),
                            reads=["widx"], writes=["%s%d" % (nm, par)])

                def emit_T(hidx):
                    e, row0, ncol = halves[hidx]
                    XT = XTs[hidx % 2]
                    for r in range(ncol // 128):
                        ld = cnt["ld"]
                        cnt["ld"] += 1
                        xb = xgt[ld % 2]
                        xn = "xgt%d" % (ld % 2)
                        tb = 6 + (ld % 2)
                        dma("sp", lambda q, xb=xb, row0=row0, r=r: q.dma_start(out=xb[:], in_=xg[row0 + r * 128:row0 + (r + 1) * 128, :]),
                            writes=[xn])
                        psb = PS[tb][:].bitcast(BF16)
                        for kc in range(8):
                            op("pe", lambda t, kc=kc, xb=xb, psb=psb: t.transpose(psb[:, kc * 128:(kc + 1) * 128], xb[:, kc * 128:(kc + 1) * 128], ident_b[:]),
                               reads=[xn, "ident_b"], writes=["ps%d" % tb])
                        op("act", lambda a, r=r, psb=psb, XT=XT: a.activation(out=XT[:, :, r * 128:(r + 1) * 128],
                                                                             in_=psb[:, :].rearrange("p (c n) -> p c n", n=128), func=AF.Copy),
                           reads=["ps%d" % tb], writes=["XT%d" % (hidx % 2)])

                def emit_GU(hidx):
                    e, row0, ncol = halves[hidx]
                    par = e % 2
                    XT, aT = XTs[hidx % 2], aTs[hidx % 2]
                    xtn, atn = "XT%d" % (hidx % 2), "aT%d" % (hidx % 2)
                    for dc in range(8):
                        gb = (dc % 2) * 2
                        sgi = cnt["sg"] % 2
                        cnt["sg"] += 1
                        sg = sgs[sgi]
                        for kc in range(8):
                            op("pe", lambda t, dc=dc, kc=kc, gb=gb: t.matmul(PS[gb][:, 0:ncol], wg[par][:, kc, dc * 128:(dc + 1) * 128], XT[:, kc, 0:ncol],
                                                                            start=(kc == 0), stop=(kc == 7)), reads=["wg%d" % par, xtn], writes=["ps%d" % gb])
                        for kc in range(8):
                            op("pe", lambda t, dc=dc, kc=kc, gb=gb: t.matmul(PS[gb + 1][:, 0:ncol], wu[par][:, kc, dc * 128:(dc + 1) * 128], XT[:, kc, 0:ncol],
                                                                            start=(kc == 0), stop=(kc == 7)), reads=["wu%d" % par, xtn], writes=["ps%d" % (gb + 1)])
                        op("act", lambda a, gb=gb, sg=sg: a.activation(out=sg[:, 0:ncol], in_=PS[gb][:, 0:ncol], func=AF.Silu), reads=["ps%d" % gb], writes=["sg%d" % sgi])
                        op("dve", lambda v, gb=gb, dc=dc, sg=sg: v.tensor_tensor(out=aT[:, dc, 0:ncol], in0=PS[gb + 1][:, 0:ncol], in1=sg[:, 0:ncol], op=ALU.mult),
                           reads=["ps%d" % (gb + 1), "sg%d" % sgi], writes=[atn])

                def emit_D(hidx):
                    e, row0, ncol = halves[hidx]
                    par = e % 2
                    aT = aTs[hidx % 2]
                    atn = "aT%d" % (hidx % 2)
                    for r in range(ncol // 128):
                        yi = cnt["yi"]
                        cnt["yi"] += 1
                        yb = yo[yi % 3]
                        yn = "yo%d" % (yi % 3)
                        for half in range(2):
                            bank = 4 + half
                            for kc in range(8):
                                op("pe", lambda t, r=r, half=half, kc=kc, bank=bank: t.matmul(
                                    PS[bank][:, :], aT[:, kc, r * 128:(r + 1) * 128], wd[par][:, kc, half * 512:(half + 1) * 512],
                                    start=(kc == 0), stop=(kc == 7)), reads=[atn, "wd%d" % par], writes=["ps%d" % bank])
                        op("act", lambda a, yb=yb: a.activation(out=yb[:, 0:512], in_=PS[4][:, :], func=AF.Copy), reads=["ps4"], writes=[yn])
                        op("dve", lambda v, yb=yb: v.tensor_copy(yb[:, 512:1024], PS[5][:, :]), reads=["ps5"], writes=[yn])
                        dma("sp", lambda q, yb=yb, row0=row0, r=r: q.dma_start(out=yg[row0 + r * 128:row0 + (r + 1) * 128, :], in_=yb[:]),
                            reads=[yn], writes=["yg_%d" % (row0 + r * 128)])

                emit_W(0)
                emit_T(0)
                for hidx, (e, row0_, ncol_) in enumerate(halves):
                    if first_of[e] == hidx and e + 1 < NE:
                        emit_W(e + 1)
                    emit_GU(hidx)
                    if hidx + 1 < len(halves):
                        emit_T(hidx + 1)
                    emit_D(hidx)
                if stop == "moe":
                    dump("yg", yg[0:3072, :], [3072, D])
                finish("moe")
                S.barrier()

            with ExitStack() as ph:
                y1 = [sb(ph, "y1_%d" % i, [128, D], F32) for i in range(2)]
                y2 = [sb(ph, "y2_%d" % i, [128, D], F32) for i in range(2)]
                xc = [sb(ph, "xc%d" % i, [128, D], F32) for i in range(2)]
                f1s = [sb(ph, "f1_%d" % i, [128, D], F32) for i in range(2)]
                zs = [sb(ph, "z_%d" % i, [128, D], F32) for i in range(2)]
                xo = [sb(ph, "xo%d" % i, [128, D], F32) for i in range(2)]
                st6s = [sb(ph, "st6c%d" % i, [128, 2, 6], F32) for i in range(2)]
                mvs = [sb(ph, "mvc%d" % i, [128, 2], F32) for i in range(2)]
                rstds = [sb(ph, "rstdc%d" % i, [128, 1], F32) for i in range(2)]
                nbs = [sb(ph, "nbc%d" % i, [128, 1], F32) for i in range(2)]
                lnr = sb(ph, "lnr2", [128, 2, D], F32)
                g2r = sb(ph, "g2r", [128, 2, D], F32)
                dma("sp", lambda q: q.dma_start(out=g2r[:], in_=g2buf[layer]), writes=["g2r"])
                dma("sp", lambda q: q.dma_start(out=lnr[:], in_=ln_rep[layer][2:4].rearrange("a p n -> p a n")), writes=["lnr"])
                for mi, stile in enumerate(q_tiles):
                    p = mi % 2
                    f1, z, st6, mv, rstd, nb = f1s[p], zs[p], st6s[p], mvs[p], rstds[p], nbs[p]
                    F1, Z, ST, MV, RS, NB = "f1_%d" % p, "z_%d" % p, "st6c%d" % p, "mvc%d" % p, "rstdc%d" % p, "nbc%d" % p
                    who = 1 if stile >= NT else 0
                    dma("pool", lambda q, mi=mi, p=p: q.indirect_dma_start(
                        out=y1[p][:], out_offset=None, in_=yg,
                        in_offset=bass.IndirectOffsetOnAxis(ap=dest_i[:, mi, 0:1].bitcast(U32), axis=0)),
                        reads=["yg", "dest_i"], writes=["y1_%d" % p])
                    dma("pool", lambda q, mi=mi, p=p: q.indirect_dma_start(
                        out=y2[p][:], out_offset=None, in_=yg,
                        in_offset=bass.IndirectOffsetOnAxis(ap=dest_i[:, mi, 1:2].bitcast(U32), axis=0)),
                        reads=["yg", "dest_i"], writes=["y2_%d" % p])
                    dma("sp", lambda q, stile=stile, p=p: q.dma_start(out=xc[p][:], in_=xbufA[stile * 128:(stile + 1) * 128, :]),
                        writes=["xc%d" % p])
                    op("dve", lambda v, mi=mi, p=p: v.tensor_scalar(f1[:], y1[p][:], wts[:, mi, 0:1], None, op0=ALU.mult),
                       reads=["y1_%d" % p, "wts"], writes=[F1])
                    op("dve", lambda v, mi=mi, p=p: v.scalar_tensor_tensor(out=f1[:], in0=y2[p][:], scalar=wts[:, mi, 1:2], in1=f1[:],
                                                                            op0=ALU.mult, op1=ALU.add), reads=["y2_%d" % p, "wts", F1], writes=[F1])
                    op("pool", lambda v: v.tensor_tensor(out=f1[:], in0=f1[:], in1=g2r[:, who, :], op=ALU.mult), reads=[F1, "g2r"], writes=[F1])
                    op("dve", lambda v, p=p: v.scalar_tensor_tensor(out=z[:], in0=xc[p][:], scalar=ALPHA, in1=f1[:], op0=ALU.mult, op1=ALU.add),
                       reads=["xc%d" % p, F1], writes=[Z])
                    for hh in range(2):
                        op("dve", lambda v, hh=hh: v.bn_stats(out=st6[:, hh, :], in_=z[:, hh * 512:(hh + 1) * 512]), reads=[Z], writes=[ST])
                    op("dve", lambda v: v.bn_aggr(out=mv[:], in_=st6[:].rearrange("p a b -> p (a b)")), reads=[ST], writes=[MV])
                    op("act", lambda a: a.activation(out=rstd[:], in_=mv[:, 1:2], func=AF.Sqrt, bias=epsc[:], scale=1.0),
                       reads=[MV, "epsc"], writes=[RS])
                    op("dve", lambda v: v.reciprocal(out=rstd[:], in_=rstd[:]), reads=[RS], writes=[RS])
                    op("dve", lambda v: v.scalar_tensor_tensor(out=nb[:], in0=mv[:, 0:1], scalar=-1.0, in1=rstd[:], op0=ALU.mult, op1=ALU.mult),
                       reads=[MV, RS], writes=[NB])
                    op("act", lambda a: a.activation(out=z[:], in_=z[:], func=AF.Identity, bias=nb[:], scale=rstd[:]),
                       reads=[Z, NB, RS], writes=[Z])
                    op("pool", lambda v: v.tensor_tensor(out=z[:], in0=z[:], in1=lnr[:, 0, :], op=ALU.mult), reads=[Z, "lnr"], writes=[Z])
                    op("dve", lambda v, p=p: v.tensor_tensor(out=xo[p][:], in0=z[:], in1=lnr[:, 1, :], op=ALU.add), reads=[Z, "lnr"], writes=["xo%d" % p])
                    if not last:
                        dma("sp", lambda q, stile=stile, p=p: q.dma_start(out=xbufB[stile * 128:(stile + 1) * 128, :], in_=xo[p][:]),
                            reads=["xo%d" % p], writes=["xbufB_%d" % stile])
                    else:
                        dma("sp", lambda q, stile=stile, p=p: q.dma_start(out=out[(stile - 2) * 128:(stile - 1) * 128, :], in_=xo[p][:]),
                            reads=["xo%d" % p], writes=["out_%d" % stile])
                if stop == "l0":
                    dump("xbufB", xbufB, [NST * 128, D])
                finish("l0")
                S.barrier()
    except _Stop:
        return nc
    S.barrier()
    top.close()
    return nc


def _host_inputs(x, c, ctx, c_ctx, w_ada, b_ada, w_in, w_pool_grp, pool_scale, w_pool_br, w_attn_br,
                 attn_sink, w_o, ln1_g, ln1_b, w_router, router_bias, w_exp_gate, w_exp_up, w_exp_down,
                 ln2_g, ln2_b):
    f32 = np.float32
    phys = [h for i in range(4) for h in (i, i + 4)]
    qcols = np.concatenate([np.arange(512 + h * 64, 512 + (h + 1) * 64) for h in phys])
    cols = np.concatenate([np.arange(0, 512), qcols, np.arange(1024, 1280), np.arange(1280, 3328)])
    w_in_p = np.ascontiguousarray(w_in[:, :, cols])
    arows = np.concatenate([np.arange(h * 64, (h + 1) * 64) for h in phys])
    w_abr_p = np.ascontiguousarray(w_attn_br[:, arows, :])
    sink_rep = np.ascontiguousarray(np.broadcast_to(attn_sink[:, None, phys], (DEPTH, 128, 8))).astype(f32)
    bada_fm = np.ascontiguousarray(b_ada[:, :2048].reshape(DEPTH, 16, 128).transpose(0, 2, 1)).astype(f32)
    bada_rep = np.ascontiguousarray(np.broadcast_to(b_ada[:, None, 2048:], (DEPTH, 128, 4 * D))).astype(f32)
    pscale = np.ascontiguousarray(pool_scale.reshape(DEPTH, 4, 128).transpose(0, 2, 1)).astype(f32)
    ln_rep = np.ascontiguousarray(np.broadcast_to(np.stack([ln1_g, ln1_b, ln2_g, ln2_b], 1)[:, :, None, :], (DEPTH, 4, 128, D))).astype(f32)
    rbias_rep = np.ascontiguousarray(np.broadcast_to(router_bias[None, :], (128, NE))).astype(f32)
    rot = np.zeros((128, 128), f32)
    for dp in range(128):
        if dp % 32 < 16:
            rot[dp + 16, dp] = -1.0
        else:
            rot[dp - 16, dp] = 1.0
    ident = np.eye(128, dtype=f32)
    kl = np.arange(128)[:, None]
    ql = np.arange(128)[None, :]
    mtri = np.stack([(kl >= ql), (kl <= ql)]).astype(f32)
    ltm = np.stack([(kl < ql), np.ones((128, 128), bool)]).astype(f32)
    eoff = np.ascontiguousarray(np.broadcast_to(np.arange(NE)[None, :], (128, NE))).astype(f32)
    slottab = np.ascontiguousarray(np.broadcast_to(np.stack([np.array(SLOT_OFF), np.array(SLOT_CAPS)], 0)[None], (128, 2, NE))).astype(f32)
    ee = np.arange(NE)
    tri32 = np.ascontiguousarray(np.broadcast_to((ee[None, :] < ee[:, None])[None], (128, NE, NE))).astype(f32)
    pk = np.concatenate([np.arange(128)[:, None], np.zeros((128, 8))], 1).astype(f32)

    def relay(w):
        return np.ascontiguousarray(w.reshape(DEPTH, NE, 8, 128, D).transpose(0, 1, 3, 2, 4)).reshape(DEPTH * NE * 128, 8 * D)
    w_eg_r, w_eu_r, w_ed_r = relay(w_exp_gate), relay(w_exp_up), relay(w_exp_down)
    dumpp = (NSLOT + np.arange(128))[:, None].astype(f32)
    inv_freq = (10000.0 ** (-np.arange(0, 32, 2, dtype=np.float32) / 32)).astype(np.float32)

    def pool_tab(t_abs, L):
        tab = np.zeros((4, len(t_abs)), f32)
        for g, w in enumerate((2, 4, 8, 16)):
            lo = np.clip(t_abs - w // 2, 0, L - 1)
            hi = np.clip(t_abs - w // 2 + w - 1, 0, L - 1)
            tab[g] = 1.0 / (hi - lo + 1)
        return tab

    in_maps = []
    for core in range(NCORE):
        b, j = core // 4, core % 4
        s = j * OWN
        t_abs = np.arange(s - HALO, s + OWN + HALO)
        valid = (t_abs >= 0) & (t_abs < L_SEQ)
        xe = np.zeros((NTOK, D), f32)
        xe[valid] = x[b, t_abs[valid]]
        tc = np.clip(t_abs, 0, L_SEQ - 1)
        row = (tc // 64).astype(np.float32)
        col = (tc % 64).astype(np.float32)
        ang = np.zeros((64, NTOK), np.float32)
        ang_r = row[None, :] * inv_freq[:, None]
        ang_c = col[None, :] * inv_freq[:, None]
        ang[0:16], ang[16:32], ang[32:48], ang[48:64] = ang_r, ang_r, ang_c, ang_c
        ropeC = np.concatenate([np.cos(ang), np.cos(ang)], 0).astype(f32)
        ropeS = np.concatenate([np.sin(ang), np.sin(ang)], 0).astype(f32)
        vmask = np.ones((128, NT + 2), f32)
        vmask[:, :NT] = valid.reshape(NT, 128)[:, 0][None, :].astype(f32)
        pinv = np.zeros((4, 128, 4, 128), f32)
        pinv[0] = pool_tab(np.clip(t_abs[256:384], 0, L_SEQ - 1), L_SEQ)[None]
        pinv[1] = pool_tab(np.clip(t_abs[(NT - 3) * 128:(NT - 2) * 128], 0, L_SEQ - 1), L_SEQ)[None]
        pinv[2] = pool_tab(np.arange(0, 128), CTX)[None]
        pinv[3] = pool_tab(np.arange(128, 256), CTX)[None]
        cc = np.stack([c[b], c_ctx], 0)
        cT2 = np.ascontiguousarray(cc.reshape(2, 8, 128).transpose(2, 1, 0)).astype(f32)
        cTrep = np.ascontiguousarray(np.broadcast_to(cc.reshape(2, 8, 128).transpose(0, 2, 1)[:, :, :, None], (2, 128, 8, 128))).astype(f32)
        in_maps.append({
            "xe": xe, "ctxin": np.ascontiguousarray(ctx[b]), "cT2": cT2, "cTrep": cTrep, "w_ada": w_ada,
            "bada_fm": bada_fm, "bada_rep": bada_rep, "w_in": w_in_p, "w_pg": w_pool_grp, "pscale": pscale,
            "w_pbr": w_pool_br, "w_abr": w_abr_p, "sink_rep": sink_rep, "w_o": w_o, "ln_rep": ln_rep,
            "w_router": w_router, "rbias_rep": rbias_rep, "w_eg": w_eg_r, "w_eu": w_eu_r, "w_ed": w_ed_r,
            "ropeC": ropeC, "ropeS": ropeS, "rotm": rot, "ident": ident, "mtri": mtri, "ltm": ltm, "vmask": vmask,
            "pinv": pinv, "eoff": eoff, "dumpp": dumpp, "slottab": slottab, "tri32": tri32, "pk": pk,
        })
    return in_maps


_NC = None


def kernel(**inputs):
    global _NC
    inputs = {k: np.asarray(v) for k, v in inputs.items()}
    in_maps = _host_inputs(**inputs)
    if _NC is None:
        _NC = build_program()
    res = run_bass_kernel_spmd(_NC, in_maps, core_ids=list(range(NCORE)))
    outs = [np.asarray(r["out"]) for r in res.results]
    full = np.stack([np.concatenate(outs[0:4], 0), np.concatenate(outs[4:8], 0)], 0)
    return full.astype(np.float32)
```

```python
from contextlib import ExitStack
import numpy as np
import concourse.bass as bass
import concourse.mybir as mybir
from concourse.bass_utils import run_bass_kernel_spmd

F32 = mybir.dt.float32
BF16 = mybir.dt.bfloat16
I32 = mybir.dt.int32
U32 = mybir.dt.uint32
AF = mybir.ActivationFunctionType
ALU = mybir.AluOpType
AX = mybir.AxisListType

D = 1024
DEPTH = 2
L_SEQ = 16384
CTX = 256
NCORE = 8
OWN = 4096
HALO = 256
NTOK = OWN + 2 * HALO
NT = NTOK // 128
NE = 32
SLOT_CAPS = [768] * 4 + [640] * 4 + [512] * 8 + [384] * 12 + [256] * 4
SLOT_OFF = [int(sum(SLOT_CAPS[:i])) for i in range(NE)]
NSLOT = int(sum(SLOT_CAPS))
ALPHA = float((2 * DEPTH) ** 0.25)
EPS = 1e-6
UPAD = 8
NDBG = 0


class _E:
    def __init__(self, name, eng, sem):
        self.name, self.eng, self.sem, self.cnt, self.known = name, eng, sem, 0, {}


class Sched:
    def __init__(self, nc, ndma=20):
        self.nc = nc
        self.E = {}
        for name, eng in (("pe", nc.tensor), ("act", nc.scalar), ("dve", nc.vector),
                          ("pool", nc.gpsimd), ("sp", nc.sync)):
            self.E[name] = _E(name, eng, nc.alloc_semaphore("c_" + name))
        self.dsem = [nc.alloc_semaphore("d%d" % i) for i in range(ndma)]
        self.dval = [0] * ndma
        self.dnext = 0
        self.last_w = {}
        self.readers = {}
        self.nops = 0
        self.rec = None

    def play(self, la, lb=()):
        assert self.rec is None
        ia = ib = 0
        na, nb_ = len(la), len(lb)
        while ia < na or ib < nb_:
            if ib >= nb_ or (ia < na and ia * max(nb_, 1) <= ib * max(na, 1)):
                it = la[ia]
                ia += 1
            else:
                it = lb[ib]
                ib += 1
            kind, eng, fn, rd, wr = it
            (self.op if kind == "op" else self.dma)(eng, fn, rd, wr)

    def _wait(self, e, tok):
        if tok[0] == "e":
            _, name, n = tok
            if name == e.name and name == "pe":
                return
            key = ("e", name)
            if e.known.get(key, 0) >= n:
                return
            e.eng.wait_ge(self.E[name].sem, n)
            e.known[key] = n
        else:
            _, si, v = tok
            key = ("d", si)
            if e.known.get(key, 0) >= v:
                return
            e.eng.wait_ge(self.dsem[si], v)
            e.known[key] = v

    def _deps(self, e, reads, writes):
        deps = []
        for r in reads:
            t = self.last_w.get(r)
            if t is not None:
                deps.append(t)
        for w in writes:
            t = self.last_w.get(w)
            if t is not None:
                deps.append(t)
            deps.extend(self.readers.get(w, ()))
        for t in deps:
            self._wait(e, t)

    def _commit(self, tok, reads, writes):
        for r in reads:
            lst = self.readers.setdefault(r, [])
            if tok[0] == "e":
                lst[:] = [x for x in lst if not (x[0] == "e" and x[1] == tok[1])]
            lst.append(tok)
        for w in writes:
            self.last_w[w] = tok
            self.readers[w] = []

    def op(self, engname, fn, reads=(), writes=()):
        if self.rec is not None:
            self.rec.append(("op", engname, fn, tuple(reads), tuple(writes)))
            return
        e = self.E[engname]
        self._deps(e, reads, writes)
        inst = fn(e.eng)
        e.cnt += 1
        inst.then_inc(e.sem, 1)
        self.nops += 1
        self._commit(("e", engname, e.cnt), reads, writes)

    def dma(self, qname, fn, reads=(), writes=()):
        if self.rec is not None:
            self.rec.append(("dma", qname, fn, tuple(reads), tuple(writes)))
            return
        e = self.E[qname]
        self._deps(e, reads, writes)
        si = self.dnext
        self.dnext = (self.dnext + 1) % len(self.dsem)
        if self.dval[si] > 0:
            self._wait(e, ("d", si, self.dval[si]))
        inst = fn(e.eng)
        self.dval[si] += 16
        inst.then_inc(self.dsem[si], 16)
        self.nops += 1
        self._commit(("d", si, self.dval[si]), reads, writes)

    def barrier(self):
        toks = [("e", n, x.cnt) for n, x in self.E.items() if x.cnt > 0]
        toks += [("d", i, v) for i, v in enumerate(self.dval) if v > 0]
        for e in self.E.values():
            for t in toks:
                if t[0] == "e" and t[1] == e.name:
                    continue
                self._wait(e, t)
        self.last_w.clear()
        self.readers.clear()


class _Stop(Exception):
    pass


def build_program(stop=None):
    nc = bass.Bass("TRN2", target_bir_lowering=False)
    dumps = []

    def dump(name, src, shape, dt=F32, rd=()):
        t = nc.dram_tensor("dbg_" + name, list(shape), dt, kind="ExternalOutput").ap()
        dumps.append((t, src, rd, name))

    def finish(tag):
        if stop != tag:
            return
        S.barrier()
        for (t, src, rd, name) in dumps:
            dma("sp", lambda q, t=t, src=src: q.dma_start(out=t, in_=src), reads=list(rd), writes=["dbg_" + name])
        S.barrier()
        raise _Stop()

    def din(name, shape, dt=F32):
        return nc.dram_tensor(name, list(shape), dt, kind="ExternalInput").ap()

    def dscr(name, shape, dt):
        return nc.dram_tensor(name, list(shape), dt, kind="Internal").ap()

    xe = din("xe", [NTOK, D])
    ctxin = din("ctxin", [CTX, D])
    cT2 = din("cT2", [128, 8, 2])
    cTrep = din("cTrep", [2, 128, 8, 128])
    w_ada = din("w_ada", [DEPTH, D, 6 * D])
    bada_fm = din("bada_fm", [DEPTH, 128, 16])
    bada_rep = din("bada_rep", [DEPTH, 128, 4 * D])
    w_in = din("w_in", [DEPTH, D, 3328])
    w_pg = din("w_pg", [DEPTH, 4, 128, 128])
    pscale = din("pscale", [DEPTH, 128, 4])
    w_pbr = din("w_pbr", [DEPTH, 512, D])
    w_abr = din("w_abr", [DEPTH, 512, D])
    sink_rep = din("sink_rep", [DEPTH, 128, 8])
    w_o = din("w_o", [DEPTH, D, D])
    ln_rep = din("ln_rep", [DEPTH, 4, 128, D])
    w_router = din("w_router", [D, NE])
    rbias_rep = din("rbias_rep", [128, NE])
    w_eg = din("w_eg", [DEPTH * NE * 128, 8 * D])
    w_eu = din("w_eu", [DEPTH * NE * 128, 8 * D])
    w_ed = din("w_ed", [DEPTH * NE * 128, 8 * D])
    ropeC = din("ropeC", [128, NTOK])
    ropeS = din("ropeS", [128, NTOK])
    rotm = din("rotm", [128, 128])
    ident_in = din("ident", [128, 128])
    mtri = din("mtri", [2, 128, 128])
    ltm = din("ltm", [2, 128, 128])
    vmask = din("vmask", [128, NT + 2])
    pinv = din("pinv", [4, 128, 4, 128])
    eoff = din("eoff", [128, NE])
    slottab = din("slottab", [128, 2, NE])
    tri32 = din("tri32", [128, NE, NE])
    pk = din("pk", [128, 9])
    dumpp = din("dumpp", [128, 1])
    out = nc.dram_tensor("out", [OWN, D], F32, kind="ExternalOutput").ap()

    NST = NT + 2
    xbufA = dscr("xbufA", [NST * 128, D], F32)
    xbufB = dscr("xbufB", [NST * 128, D], F32)
    UW = UPAD + NTOK + UPAD
    ubuf = dscr("ubuf", [512, UW], BF16)
    UWC = UPAD + CTX + UPAD
    ubufc = dscr("ubufc", [512, UWC], BF16)
    xg = dscr("xg", [NSLOT + 128, D], BF16)
    yg = dscr("yg", [NSLOT + 128, D], F32)
    g2buf = dscr("g2buf", [DEPTH, 128, 2, D], F32)
    sdram = dscr("sdram", [128, 36, NE], F32)
    hbuf = dscr("hbuf", [36 * 128, D], BF16)

    S = Sched(nc)
    op, dma = S.op, S.dma

    uid = [0]

    def sb(stack, name, shape, dt):
        uid[0] += 1
        return stack.enter_context(nc.sbuf_tensor("%s_%d" % (name, uid[0]), list(shape), dt))

    top = ExitStack()
    PS = [top.enter_context(nc.psum_tensor("psb%d" % i, [128, 512], F32)) for i in range(8)]

    ident_f = sb(top, "ident_f", [128, 128], F32)
    ident_b = sb(top, "ident_b", [128, 128], BF16)
    rot_b = sb(top, "rot_b", [128, 128], BF16)
    mtri_b = sb(top, "mtri_b", [128, 2, 128], BF16)
    lt_b = sb(top, "lt_b", [128, 2, 128], BF16)
    vm = sb(top, "vm", [128, NT + 2], F32)
    eoff_s = sb(top, "eoff_s", [128, NE], F32)
    dumpp_s = sb(top, "dumpp_s", [128, 1], F32)
    rbias_s = sb(top, "rbias_s", [128, NE], F32)
    wr_s = sb(top, "wr_s", [128, 8, NE], F32)
    zero_b = sb(top, "zero_b", [128, 16], BF16)
    zero_f = sb(top, "zero_f", [128, 64], F32)
    epsc = sb(top, "epsc", [128, 1], F32)
    dest_i = sb(top, "dest_i", [128, 36, 2], I32)
    wts = sb(top, "wts", [128, 36, 2], F32)
    widx = sb(top, "widx", [128, NE, 8], I32)
    pk_s = sb(top, "pk_s", [128, 9], F32)
    slot_s = sb(top, "slot_s", [128, 2, NE], F32)

    dma("sp", lambda q: q.dma_start(out=ident_f[:], in_=ident_in), writes=["ident_f"])
    dma("pool", lambda q: q.dma_start(out=ident_b[:], in_=ident_in), writes=["ident_b"])
    dma("pool", lambda q: q.dma_start(out=rot_b[:], in_=rotm), writes=["rot_b"])
    dma("pool", lambda q: q.dma_start(out=mtri_b[:], in_=mtri.rearrange("a p n -> p a n")), writes=["mtri_b"])
    dma("pool", lambda q: q.dma_start(out=lt_b[:], in_=ltm.rearrange("a p n -> p a n")), writes=["lt_b"])
    dma("sp", lambda q: q.dma_start(out=vm[:], in_=vmask), writes=["vm"])
    dma("sp", lambda q: q.dma_start(out=eoff_s[:], in_=eoff), writes=["eoff_s"])
    dma("sp", lambda q: q.dma_start(out=dumpp_s[:], in_=dumpp), writes=["dumpp_s"])
    dma("sp", lambda q: q.dma_start(out=pk_s[:], in_=pk), writes=["pk_s"])
    dma("sp", lambda q: q.dma_start(out=slot_s[:], in_=slottab), writes=["slot_s"])
    dma("sp", lambda q: q.dma_start(out=rbias_s[:], in_=rbias_rep), writes=["rbias_s"])
    dma("sp", lambda q: q.dma_start(out=wr_s[:], in_=w_router.rearrange("(kc p) n -> p kc n", p=128)), writes=["wr_s"])
    op("dve", lambda v: v.memset(zero_b[:], 0.0), writes=["zero_b"])
    op("dve", lambda v: v.memset(zero_f[:], 0.0), writes=["zero_f"])
    op("dve", lambda v: v.memset(epsc[:], EPS), writes=["epsc"])
    for g in range(4):
        for (buf, w, nm) in ((ubuf, UW, "ubuf"), (ubufc, UWC, "ubufc")):
            dma("sp", lambda q, buf=buf, g=g: q.dma_start(out=buf[g * 128:(g + 1) * 128, 0:UPAD], in_=zero_b[:, 0:UPAD]),
                reads=["zero_b"], writes=[nm + "_padl"])
            dma("sp", lambda q, buf=buf, g=g, w=w: q.dma_start(out=buf[g * 128:(g + 1) * 128, w - UPAD:w], in_=zero_b[:, 0:UPAD]),
                reads=["zero_b"], writes=[nm + "_padr"])
    for qd in range(16):
        dma("sp", lambda q, qd=qd: q.dma_start(out=yg[NSLOT:NSLOT + 128, qd * 64:(qd + 1) * 64], in_=zero_f[:]),
            reads=["zero_f"], writes=["yg_dump%d" % qd])

    def xsrc(layer, st):
        if layer == 0:
            if st < NT:
                return xe[st * 128:(st + 1) * 128, :]
            return ctxin[(st - NT) * 128:(st - NT + 1) * 128, :]
        return xbufB[st * 128:(st + 1) * 128, :]

    try:
      for layer in range(DEPTH):
        last = layer == DEPTH - 1
        if layer == 0:
            kv_tiles = list(range(NT)) + [NT, NT + 1]
            q_tiles = list(range(1, NT - 1)) + [NT, NT + 1]
        else:
            kv_tiles = list(range(1, NT - 1)) + [NT, NT + 1]
            q_tiles = list(range(2, NT - 2))
        n_moe = len(q_tiles)

        with ExitStack() as lay:
            mod_fm = sb(lay, "mod_fm", [128, 16, 2], F32)
            rep = sb(lay, "rep", [128, 2, 3, D], F32)
            with ExitStack() as ph:
                sc_f = sb(ph, "sc_f", [128, 8, 2], F32)
                sc_rep = sb(ph, "sc_rep", [128, 2, 8, 128], F32)
                bfm = sb(ph, "bfm", [128, 16], F32)
                brep = sb(ph, "brep", [128, 4 * D], F32)
                g2t = sb(ph, "g2t", [128, 2, D], F32)
                wa = [sb(ph, "wa%d" % i, [128, 8, 512], F32) for i in range(2)]
                dma("sp", lambda q: q.dma_start(out=sc_f[:], in_=cT2), writes=["sc_f"])
                dma("sp", lambda q: q.dma_start(out=sc_rep[:], in_=cTrep.rearrange("a p k m -> p a k m")), writes=["sc_rep"])
                dma("sp", lambda q: q.dma_start(out=bfm[:], in_=bada_fm[layer]), writes=["bfm"])
                dma("sp", lambda q: q.dma_start(out=brep[:], in_=bada_rep[layer]), writes=["brep"])
                op("act", lambda a: a.activation(out=sc_f[:], in_=sc_f[:], func=AF.Silu), reads=["sc_f"], writes=["sc_f"])
                op("act", lambda a: a.activation(out=sc_rep[:], in_=sc_rep[:], func=AF.Silu), reads=["sc_rep"], writes=["sc_rep"])
                for blk in range(12):
                    wb = wa[blk % 2]
                    wn = "wa%d" % (blk % 2)
                    dma("sp", lambda q, wb=wb, blk=blk: q.dma_start(
                        out=wb[:], in_=w_ada[layer][:, blk * 512:(blk + 1) * 512].rearrange("(kc p) n -> p kc n", p=128)),
                        writes=[wn])
                    if blk < 4:
                        for j in range(4):
                            fc = blk * 4 + j
                            for kc in range(8):
                                op("pe", lambda t, wb=wb, j=j, kc=kc, fc=fc: t.matmul(
                                    PS[0][:, fc * 2:fc * 2 + 2], wb[:, kc, j * 128:(j + 1) * 128], sc_f[:, kc, :],
                                    start=(kc == 0), stop=(kc == 7)), reads=[wn, "sc_f"], writes=["ps0"])
                    else:
                        ch = (blk - 4) // 2
                        half = (blk - 4) % 2
                        for who in range(2):
                            bank = 1 + who
                            for kc in range(8):
                                op("pe", lambda t, wb=wb, kc=kc, who=who, bank=bank: t.matmul(
                                    PS[bank][:, :], sc_rep[:, who, kc, :], wb[:, kc, :],
                                    start=(kc == 0), stop=(kc == 7)), reads=[wn, "sc_rep"], writes=["ps%d" % bank])
                            dst_ = rep[:, who, ch, half * 512:(half + 1) * 512] if ch < 3 else g2t[:, who, half * 512:(half + 1) * 512]
                            op("dve", lambda v, who=who, bank=bank, ch=ch, half=half, dst_=dst_: v.tensor_tensor(
                                out=dst_, in0=PS[bank][:, :],
                                in1=brep[:, ch * D + half * 512: ch * D + (half + 1) * 512], op=ALU.add),
                                reads=["ps%d" % bank, "brep"], writes=["rep" if ch < 3 else "g2t"])
                for who in range(2):
                    op("dve", lambda v, who=who: v.tensor_tensor(
                        out=mod_fm[:, :, who], in0=PS[0][:, 0:32].rearrange("p (f w) -> p f w", w=2)[:, :, who],
                        in1=bfm[:, :], op=ALU.add), reads=["ps0", "bfm"], writes=["mod_fm"])
                op("dve", lambda v: v.tensor_scalar(mod_fm[:, 8:16, :], mod_fm[:, 8:16, :], 1.0, None, op0=ALU.add),
                   reads=["mod_fm"], writes=["mod_fm"])
                for who in range(2):
                    op("dve", lambda v, who=who: v.tensor_scalar(rep[:, who, 2, :], rep[:, who, 2, :], 1.0, None, op0=ALU.add),
                       reads=["rep"], writes=["rep"])
                dma("sp", lambda q: q.dma_start(out=g2buf[layer], in_=g2t[:]), reads=["g2t"], writes=["g2buf"])
                if stop == "ada":
                    dump("rep", rep[:].rearrange("p a b n -> p (a b n)"), [128, 6 * D])
                    dump("modfm", mod_fm[:].rearrange("p a b -> p (a b)"), [128, 32])
                finish("ada")
                S.barrier()

            with ExitStack() as ph:
                win = sb(ph, "win", [128, 8, 3328], BF16)
                wpg = sb(ph, "wpg", [128, 4, 128], BF16)
                psc = sb(ph, "psc", [128, 4], F32)
                wpb = sb(ph, "wpb", [128, 4, D], BF16)
                wab = sb(ph, "wab", [128, 4, D], BF16)
                wo = sb(ph, "wo", [128, 8, D], BF16)
                esink = sb(ph, "esink", [128, 8], F32)
                lnr = sb(ph, "lnr1", [128, 2, D], F32)
                dma("sp", lambda q: q.dma_start(out=lnr[:], in_=ln_rep[layer][0:2].rearrange("a p n -> p a n")), writes=["lnr"])
                for kc in range(8):
                    dma("pool", lambda q, kc=kc: q.dma_start(out=win[:, kc, :], in_=w_in[layer][kc * 128:(kc + 1) * 128, :]),
                        writes=["win"])
                dma("pool", lambda q: q.dma_start(out=wpg[:], in_=w_pg[layer].rearrange("g c d -> c g d")), writes=["wpg"])
                dma("sp", lambda q: q.dma_start(out=psc[:], in_=pscale[layer]), writes=["psc"])
                dma("pool", lambda q: q.dma_start(out=wpb[:], in_=w_pbr[layer].rearrange("(kc p) n -> p kc n", p=128)), writes=["wpb"])
                dma("pool", lambda q: q.dma_start(out=wab[:], in_=w_abr[layer].rearrange("(kc p) n -> p kc n", p=128)), writes=["wab"])
                dma("pool", lambda q: q.dma_start(out=wo[:], in_=w_o[layer].rearrange("(kc p) n -> p kc n", p=128)), writes=["wo"])
                dma("sp", lambda q: q.dma_start(out=esink[:], in_=sink_rep[layer]), writes=["esink"])
                op("act", lambda a: a.activation(out=esink[:], in_=esink[:], func=AF.Exp), reads=["esink"], writes=["esink"])

                kT = sb(ph, "kT", [128, NTOK + CTX], BF16)
                vA = sb(ph, "vA", [128, NT + 2, 2, 65], BF16)

                xt = [sb(ph, "xt%d" % i, [128, D], F32) for i in range(2)]
                m1 = sb(ph, "m1", [128, D], F32)
                m2 = sb(ph, "m2", [128, D], F32)
                yt = sb(ph, "yt", [128, D], F32)
                hT = sb(ph, "hT", [128, 8, 128], BF16)
                st6 = sb(ph, "st6", [128, 2, 6], F32)
                mv = sb(ph, "mv", [128, 2], F32)
                rstd = sb(ph, "rstd", [128, 1], F32)
                nb = sb(ph, "nb", [128, 1], F32)
                ropc = sb(ph, "ropc", [128, 128], F32)
                rops = sb(ph, "rops", [128, 128], F32)
                qk_sb = sb(ph, "qk_sb", [128, 5, 128], BF16)
                r1 = sb(ph, "r1", [128, 128], F32)
                r2 = sb(ph, "r2", [128, 128], F32)
                uT_t = sb(ph, "uT_t", [128, 4, 128], BF16)

                lnA = dict(st6=st6, mv=mv, rstd=rstd, nb=nb, sfx="")
                lnB = dict(st6=sb(ph, "st6B", [128, 2, 6], F32), mv=sb(ph, "mvB", [128, 2], F32),
                           rstd=sb(ph, "rstdB", [128, 1], F32), nb=sb(ph, "nbB", [128, 1], F32), sfx="B")

                def ln_stats(src, srcname, sc=None):
                    sc = sc or lnA
                    st6, mv, rstd, nb, sfx = sc["st6"], sc["mv"], sc["rstd"], sc["nb"], sc["sfx"]
                    for hh in range(2):
                        op("dve", lambda v, hh=hh: v.bn_stats(out=st6[:, hh, :], in_=src[:, hh * 512:(hh + 1) * 512]),
                           reads=[srcname], writes=["st6" + sfx])
                    op("dve", lambda v: v.bn_aggr(out=mv[:], in_=st6[:].rearrange("p a b -> p (a b)")), reads=["st6" + sfx], writes=["mv" + sfx])
                    op("act", lambda a: a.activation(out=rstd[:], in_=mv[:, 1:2], func=AF.Sqrt, bias=epsc[:], scale=1.0),
                       reads=["mv" + sfx, "epsc"], writes=["rstd" + sfx])
                    op("dve", lambda v: v.reciprocal(out=rstd[:], in_=rstd[:]), reads=["rstd" + sfx], writes=["rstd" + sfx])
                    op("dve", lambda v: v.scalar_tensor_tensor(out=nb[:], in0=mv[:, 0:1], scalar=-1.0, in1=rstd[:],
                                                               op0=ALU.mult, op1=ALU.mult), reads=["mv" + sfx, "rstd" + sfx], writes=["nb" + sfx])

                def make_hT(layer, stile, xbuf, xname, yt_=None, ytn="yt", hT_=None, hTn="hT", sc=None, banks=(0, 1)):
                    yt_ = yt if yt_ is None else yt_
                    hT_ = hT if hT_ is None else hT_
                    sc = sc or lnA
                    who = 0 if stile < NT else 1
                    ln_stats(xbuf, xname, sc)
                    op("act", lambda a: a.activation(out=yt_[:], in_=xbuf[:], func=AF.Identity, bias=sc["nb"][:], scale=sc["rstd"][:]),
                       reads=[xname, "nb" + sc["sfx"], "rstd" + sc["sfx"]], writes=[ytn])
                    for half in range(2):
                        bk = banks[half]
                        for j in range(4):
                            kc = half * 4 + j
                            op("pe", lambda t, kc=kc, j=j, bk=bk: t.transpose(
                                PS[bk][:, j * 128:(j + 1) * 128], yt_[:, kc * 128:(kc + 1) * 128], ident_f[:]),
                                reads=[ytn, "ident_f"], writes=["ps%d" % bk])
                        for j in range(4):
                            kc = half * 4 + j
                            op("act", lambda a, kc=kc, j=j, bk=bk: a.activation(
                                out=hT_[:, kc, :], in_=PS[bk][:, j * 128:(j + 1) * 128], func=AF.Identity,
                                bias=mod_fm[:, kc, who:who + 1], scale=mod_fm[:, 8 + kc, who:who + 1]),
                                reads=["ps%d" % bk, "mod_fm"], writes=[hTn])

                def rope(dst, dstname, nch, tok0):
                    assert nch == 4
                    dma("sp", lambda q: q.dma_start(out=ropc[:], in_=ropeC[:, tok0:tok0 + 128]), writes=["ropc"])
                    dma("sp", lambda q: q.dma_start(out=rops[:], in_=ropeS[:, tok0:tok0 + 128]), writes=["rops"])
                    for c in range(4):
                        op("pe", lambda t, c=c: t.matmul(PS[3][:, c * 128:(c + 1) * 128], rot_b[:], qk_sb[:, c, :], start=True, stop=True),
                           reads=["rot_b", "qk_sb"], writes=["ps3"])
                    cb = ropc[:].unsqueeze(1).to_broadcast([128, 4, 128])
                    sbb = rops[:].unsqueeze(1).to_broadcast([128, 4, 128])
                    rq1 = yt[:, 0:512].rearrange("p (c n) -> p c n", n=128)
                    rq2 = yt[:, 512:1024].rearrange("p (c n) -> p c n", n=128)
                    op("dve", lambda v: v.tensor_tensor(out=rq1, in0=qk_sb[:, 0:4, :], in1=cb, op=ALU.mult),
                       reads=["qk_sb", "ropc"], writes=["yt"])
                    op("dve", lambda v: v.tensor_tensor(out=rq2, in0=PS[3][:, :].rearrange("p (c n) -> p c n", n=128), in1=sbb, op=ALU.mult),
                       reads=["ps3", "rops"], writes=["yt"])
                    op("dve", lambda v: v.tensor_tensor(out=dst, in0=rq1, in1=rq2, op=ALU.add),
                       reads=["yt"], writes=[dstname])

                gates2 = [sb(ph, "gates%d" % i, [128, 2048], BF16) for i in range(2)]
                qT = sb(ph, "qT", [128, 4, 128], BF16)
                uwin = sb(ph, "uwin", [128, 4, 128 + 2 * UPAD], BF16)
                s2 = sb(ph, "s2", [128, 144], F32)
                s4 = sb(ph, "s4", [128, 144], F32)
                s8 = sb(ph, "s8", [128, 144], F32)
                s16 = sb(ph, "s16", [128, 144], F32)
                ptab = sb(ph, "ptab", [128, 4, 128], F32)
                pooled = sb(ph, "pooled", [128, 4, 128], BF16)
                poT2 = [sb(ph, "poT%d" % i, [128, 4, 128], BF16) for i in range(2)]
                PT = sb(ph, "PT", [128, 5, 8, 128], BF16)
                den = sb(ph, "den", [128, 8], F32)
                ao2 = [sb(ph, "ao%d" % i, [128, 512], BF16) for i in range(2)]
                aoT = sb(ph, "aoT", [128, 4, 128], BF16)
                mb = sb(ph, "mb", [128, D], BF16)
                mT = sb(ph, "mT", [128, 8, 128], BF16)
                h2T = sb(ph, "h2T", [128, 8, 128], F32)
                s_t = sb(ph, "s_t", [128, NE], F32)

                def pass1_tile(i, stile):
                    p_ = i % 2
                    xb = xt[p_]
                    xn = "xt%d" % p_
                    isctx = stile >= NT
                    if p_ == 0:
                        yt_, ytn, hT_, hTn, sc, banks, ub_bank, kv_bank = yt, "yt", hT, "hT", lnA, (0, 1), 2, 5
                        uT_, uTn, qch, r1_, r1n, r2_, r2n = uT_t, "uT_t", 4, r1[:], "r1", r2[:], "r2"
                        rc_, rcn, rs_, rsn = ropc[:], "ropc", rops[:], "rops"
                    else:
                        yt_, ytn, hT_, hTn, sc, banks, ub_bank, kv_bank = m1, "m1", mT, "mT", lnB, (3, 7), 4, 6
                        uT_, uTn, qch, r1_, r1n, r2_, r2n = pooled, "pooled", 3, s2[:, 0:128], "s2", s4[:, 0:128], "s4"
                        rc_, rcn, rs_, rsn = s8[:, 0:128], "s8", s16[:, 0:128], "s16"
                    un, kvn, qn = "ps%d" % ub_bank, "ps%d" % kv_bank, "qk_sb%d" % qch
                    recF, recK = [], []
                    S.rec = recF
                    dma("sp", lambda q: q.dma_start(out=xb[:], in_=xsrc(layer, stile)), writes=[xn])
                    make_hT(layer, stile, xb, xn, yt_, ytn, hT_, hTn, sc, banks)
                    S.rec = recK
                    vms = vm[:, stile:stile + 1]
                    kcol = stile * 128 if not isctx else NTOK + (stile - NT) * 128
                    for g in range(4):
                        for kc in range(8):
                            op("pe", lambda t, g=g, kc=kc: t.matmul(PS[ub_bank][:, g * 128:(g + 1) * 128], win[:, kc, g * 128:(g + 1) * 128],
                                                                   hT_[:, kc, :], start=(kc == 0), stop=(kc == 7)),
                               reads=["win", hTn], writes=[un])
                    for kc in range(8):
                        op("pe", lambda t, kc=kc: t.matmul(PS[kv_bank][:, 0:128], win[:, kc, 1024:1152], hT_[:, kc, :],
                                                           start=(kc == 0), stop=(kc == 7)), reads=["win", hTn], writes=[kvn])
                    for kc in range(8):
                        op("pe", lambda t, kc=kc: t.matmul(PS[kv_bank][:, 128:256], hT_[:, kc, :], win[:, kc, 1152:1280],
                                                           start=(kc == 0), stop=(kc == 7)), reads=["win", hTn], writes=[kvn])
                    op("act", lambda a: a.activation(out=uT_[:].rearrange("p g n -> p (g n)"), in_=PS[ub_bank][:, :],
                                                     func=AF.Copy, scale=vms), reads=[un, "vm"], writes=[uTn])
                    ub, uoff, unm = (ubuf, UPAD + stile * 128, "ubuf") if not isctx else (ubufc, UPAD + (stile - NT) * 128, "ubufc")
                    for g in range(4):
                        dma("sp", lambda q, g=g: q.dma_start(out=ub[g * 128:(g + 1) * 128, uoff:uoff + 128], in_=uT_[:, g, :]),
                            reads=[uTn], writes=[unm + "_%d" % stile])
                    op("act", lambda a: a.activation(
                        out=vA[:, stile, :, 0:64], in_=PS[kv_bank][:, 128:256].rearrange("p (h d) -> p h d", d=64), func=AF.Copy, scale=vms),
                        reads=[kvn, "vm"], writes=["vA_%d" % stile])
                    for hh in range(2):
                        op("dve", lambda v, hh=hh: v.tensor_copy(vA[:, stile, hh, 64:65], vms), reads=["vm"], writes=["vA_%d" % stile])
                    if isctx:
                        op("act", lambda a: a.activation(out=kT[:, kcol:kcol + 128], in_=PS[kv_bank][:, 0:128], func=AF.Copy),
                           reads=[kvn], writes=["kT_%d" % stile])
                    else:
                        op("act", lambda a: a.activation(out=qk_sb[:, qch, :], in_=PS[kv_bank][:, 0:128], func=AF.Copy, scale=vms),
                           reads=[kvn, "vm"], writes=[qn])
                        dma("sp", lambda q: q.dma_start(out=rc_, in_=ropeC[:, kcol:kcol + 128]), writes=[rcn])
                        dma("sp", lambda q: q.dma_start(out=rs_, in_=ropeS[:, kcol:kcol + 128]), writes=[rsn])
                        op("pe", lambda t: t.matmul(PS[kv_bank][:, 256:384], rot_b[:], qk_sb[:, qch, :], start=True, stop=True),
                           reads=["rot_b", qn], writes=[kvn])
                        op("dve", lambda v: v.tensor_tensor(out=r1_, in0=qk_sb[:, qch, :], in1=rc_, op=ALU.mult), reads=[qn, rcn], writes=[r1n])
                        op("dve", lambda v: v.tensor_tensor(out=r2_, in0=PS[kv_bank][:, 256:384], in1=rs_, op=ALU.mult), reads=[kvn, rsn], writes=[r2n])
                        op("dve", lambda v: v.tensor_tensor(out=kT[:, kcol:kcol + 128], in0=r1_, in1=r2_, op=ALU.add),
                           reads=[r1n, r2n], writes=["kT_%d" % stile])
                    S.rec = None
                    p1F.append(recF)
                    p1K.append(recK)

                p1F, p1K = [], []
                for i_, stile_ in enumerate(kv_tiles):
                    pass1_tile(i_, stile_)
                S.play(p1F[0])
                for i_ in range(len(p1K)):
                    S.play(p1F[i_ + 1] if i_ + 1 < len(p1F) else [], p1K[i_])
                if stop == "p1":
                    dump("kT", kT[:], [128, NTOK + CTX], BF16)
                    dump("vA", vA[:].rearrange("p a b c -> p (a b c)"), [128, (NT + 2) * 130], BF16)
                    dump("ubuf", ubuf, [512, UW], BF16)
                    dump("ubufc", ubufc, [512, UWC], BF16)
                finish("p1")
                S.barrier()
                allA, allB = [], []
                def tile_body(mi, stile):
                    isctx = stile >= NT
                    who = 1 if isctx else 0
                    xb = xt[mi % 2]
                    xn = "xt%d" % (mi % 2)
                    gates = gates2[mi % 2]
                    ao = ao2[mi % 2]
                    poT = poT2[mi % 2]
                    gn, aon, pon = "gates%d" % (mi % 2), "ao%d" % (mi % 2), "poT%d" % (mi % 2)
                    recA = []
                    S.rec = recA
                    dma("sp", lambda q, xb=xb, stile=stile: q.dma_start(out=xb[:], in_=xsrc(layer, stile)), writes=[xn])
                    ub, uoff = (ubuf, stile * 128) if not isctx else (ubufc, (stile - NT) * 128)
                    ureads = (["ubuf_%d" % t for t in (stile - 1, stile, stile + 1)] + ["ubuf_padl", "ubuf_padr"]) if not isctx else \
                        ["ubufc_%d" % NT, "ubufc_%d" % (NT + 1), "ubufc_padl", "ubufc_padr"]
                    for g in range(4):
                        dma("sp", lambda q, g=g, ub=ub, uoff=uoff: q.dma_start(out=uwin[:, g, :], in_=ub[g * 128:(g + 1) * 128, uoff:uoff + 144]),
                            reads=ureads, writes=["uwin"])
                    make_hT(layer, stile, xb, xn)
                    for c in range(4):
                        for kc in range(8):
                            op("pe", lambda t, c=c, kc=kc: t.matmul(PS[2][:, c * 128:(c + 1) * 128], win[:, kc, 512 + c * 128:512 + (c + 1) * 128],
                                                                   hT[:, kc, :], start=(kc == 0), stop=(kc == 7)),
                               reads=["win", "hT"], writes=["ps2"])
                    if isctx:
                        op("act", lambda a: a.activation(out=qT[:].rearrange("p c n -> p (c n)"), in_=PS[2][:, :], func=AF.Copy),
                           reads=["ps2"], writes=["qT"])
                    else:
                        op("act", lambda a: a.activation(out=qk_sb[:, 0:4, :].rearrange("p c n -> p (c n)"), in_=PS[2][:, :], func=AF.Copy),
                           reads=["ps2"], writes=["qk_sb"])
                        rope(qT[:], "qT", 4, stile * 128)
                    for blk in range(4):
                        bank = blk % 2
                        for kc in range(8):
                            op("pe", lambda t, blk=blk, kc=kc, bank=bank: t.matmul(
                                PS[bank][:, :], hT[:, kc, :], win[:, kc, 1280 + blk * 512:1280 + (blk + 1) * 512],
                                start=(kc == 0), stop=(kc == 7)), reads=["win", "hT"], writes=["ps%d" % bank])
                        op("act", lambda a, blk=blk, bank=bank: a.activation(out=gates[:, blk * 512:(blk + 1) * 512], in_=PS[bank][:, :],
                                                                             func=AF.Sigmoid), reads=["ps%d" % bank], writes=[gn])
                    edge = None
                    if isctx:
                        edge = 2 + (stile - NT)
                    elif stile == 2:
                        edge = 0
                    elif stile == NT - 3:
                        edge = 1
                    if edge is not None:
                        dma("sp", lambda q, edge=edge: q.dma_start(out=ptab[:], in_=pinv[edge]), writes=["ptab"])
                    for g in range(4):
                        U = uwin[:, g, :]
                        op("dve", lambda v, U=U: v.tensor_tensor(out=s2[:, 1:144], in0=U[:, 0:143], in1=U[:, 1:144], op=ALU.add),
                           reads=["uwin"], writes=["s2"])
                        cur, curname = s2, "s2"
                        if g >= 1:
                            op("dve", lambda v: v.tensor_tensor(out=s4[:, 2:143], in0=s2[:, 1:142], in1=s2[:, 3:144], op=ALU.add),
                               reads=["s2"], writes=["s4"])
                            cur, curname = s4, "s4"
                        if g >= 2:
                            op("dve", lambda v: v.tensor_tensor(out=s8[:, 4:141], in0=s4[:, 2:139], in1=s4[:, 6:143], op=ALU.add),
                               reads=["s4"], writes=["s8"])
                            cur, curname = s8, "s8"
                        if g >= 3:
                            op("dve", lambda v: v.tensor_tensor(out=s16[:, 8:137], in0=s8[:, 4:133], in1=s8[:, 12:141], op=ALU.add),
                               reads=["s8"], writes=["s16"])
                            cur, curname = s16, "s16"
                        wdw = (2, 4, 8, 16)[g]
                        if edge is None:
                            op("dve", lambda v, g=g, cur=cur, U=U, wdw=wdw: v.scalar_tensor_tensor(
                                out=pooled[:, g, :], in0=cur[:, 8:136], scalar=1.0 / wdw, in1=U[:, 8:136], op0=ALU.mult, op1=ALU.subtract),
                                reads=[curname, "uwin"], writes=["pooled"])
                        else:
                            op("dve", lambda v, g=g, cur=cur: v.tensor_tensor(out=r1[:], in0=cur[:, 8:136], in1=ptab[:, g, :], op=ALU.mult),
                               reads=[curname, "ptab"], writes=["r1"])
                            op("dve", lambda v, g=g, U=U: v.tensor_tensor(out=pooled[:, g, :], in0=r1[:], in1=U[:, 8:136], op=ALU.subtract),
                               reads=["r1", "uwin"], writes=["pooled"])
                    for g in range(4):
                        op("pe", lambda t, g=g: t.matmul(PS[2][:, g * 128:(g + 1) * 128], wpg[:, g, :], pooled[:, g, :], start=True, stop=True),
                           reads=["wpg", "pooled"], writes=["ps2"])
                    for g in range(4):
                        op("act", lambda a, g=g: a.activation(out=poT[:, g, :], in_=PS[2][:, g * 128:(g + 1) * 128], func=AF.Copy,
                                                              scale=psc[:, g:g + 1]), reads=["ps2", "psc"], writes=[pon])
                    if isctx:
                        kblocks = [(NTOK, NT, None), (NTOK + 128, NT + 1, None)]
                    else:
                        kblocks = [((stile - 1) * 128, stile - 1, 0), (stile * 128, stile, None), ((stile + 1) * 128, stile + 1, 1),
                                   (NTOK, NT, None), (NTOK + 128, NT + 1, None)]
                    nkb = len(kblocks)
                    for bi, (kc0, vt, msk) in enumerate(kblocks):
                        for ph_ in range(8):
                            c, half = ph_ // 2, ph_ % 2
                            bank = 3 + half
                            op("pe", lambda t, c=c, half=half, bank=bank, ph_=ph_, kc0=kc0: t.matmul(
                                PS[bank][:, c * 128:(c + 1) * 128],
                                kT[half * 64:(half + 1) * 64, kc0:kc0 + 128], qT[half * 64:(half + 1) * 64, c, :],
                                start=True, stop=True), reads=["kT", "qT"], writes=["ps%d" % bank])
                        for hb in range(2):
                            op("act", lambda a, hb=hb, bi=bi: a.activation(
                                out=PT[:, bi, hb * 4:(hb + 1) * 4, :].rearrange("p h n -> p (h n)"), in_=PS[3 + hb][:, :],
                                func=AF.Exp, scale=0.125), reads=["ps%d" % (3 + hb)], writes=["PT"])
                        if msk is not None:
                            for ph_ in range(8):
                                op("pool", lambda v, bi=bi, ph_=ph_, msk=msk: v.tensor_tensor(
                                    out=PT[:, bi, ph_, :], in0=PT[:, bi, ph_, :], in1=mtri_b[:, msk, :], op=ALU.mult),
                                    reads=["PT", "mtri_b"], writes=["PT"])
                    for ph_ in range(8):
                        half = ph_ % 2
                        bank = ph_ // 4
                        for bi, (kc0, vt, msk) in enumerate(kblocks):
                            op("pe", lambda t, ph_=ph_, half=half, bank=bank, bi=bi, vt=vt: t.matmul(
                                PS[bank][:, (ph_ % 4) * 65:(ph_ % 4) * 65 + 65], PT[:, bi, (ph_ % 2) * 4 + ph_ // 2, :], vA[:, vt, half, :],
                                start=(bi == 0), stop=(bi == nkb - 1)), reads=["PT", "vA"], writes=["ps%d" % bank])
                    for hb in range(2):
                        op("dve", lambda v, hb=hb: v.tensor_tensor(
                            out=den[:, hb * 4:(hb + 1) * 4], in0=PS[hb][:, 0:260].rearrange("p (h d) -> p h d", d=65)[:, :, 64],
                            in1=esink[:, hb * 4:(hb + 1) * 4], op=ALU.add), reads=["ps%d" % hb, "esink"], writes=["den"])
                    op("dve", lambda v: v.reciprocal(out=den[:], in_=den[:]), reads=["den"], writes=["den"])
                    for hb in range(2):
                        op("dve", lambda v, hb=hb: v.tensor_tensor(
                            out=ao[:, hb * 256:(hb + 1) * 256].rearrange("p (h d) -> p h d", d=64),
                            in0=PS[hb][:, 0:260].rearrange("p (h d) -> p h d", d=65)[:, :, 0:64],
                            in1=den[:, hb * 4:(hb + 1) * 4].unsqueeze(2).to_broadcast([128, 4, 64]), op=ALU.mult),
                            reads=["ps%d" % hb, "den"], writes=[aon])
                    recB = []
                    S.rec = recB
                    psb = PS[5][:].bitcast(BF16)
                    for c in range(4):
                        op("pe", lambda t, c=c, psb=psb, ao=ao: t.transpose(psb[:, c * 128:(c + 1) * 128], ao[:, c * 128:(c + 1) * 128], ident_b[:]),
                           reads=[aon, "ident_b"], writes=["ps5"])
                    op("act", lambda a, psb=psb: a.activation(out=aoT[:].rearrange("p c n -> p (c n)"), in_=psb[:, 0:512], func=AF.Copy),
                       reads=["ps5"], writes=["aoT"])
                    for half in range(2):
                        for kc in range(4):
                            op("pe", lambda t, half=half, kc=kc: t.matmul(PS[5][:, :], poT[:, kc, :], wpb[:, kc, half * 512:(half + 1) * 512],
                                                                         start=(kc == 0), stop=(kc == 3)), reads=[pon, "wpb"], writes=["ps5"])
                        for kc in range(4):
                            op("pe", lambda t, half=half, kc=kc: t.matmul(PS[6][:, :], aoT[:, kc, :], wab[:, kc, half * 512:(half + 1) * 512],
                                                                         start=(kc == 0), stop=(kc == 3)), reads=["aoT", "wab"], writes=["ps6"])
                        op("dve", lambda v, half=half: v.tensor_tensor(out=m1[:, half * 512:(half + 1) * 512], in0=PS[5][:, :],
                                                                       in1=gates[:, half * 512:(half + 1) * 512], op=ALU.mult),
                           reads=["ps5", gn], writes=["m1"])
                        op("dve", lambda v, half=half: v.tensor_tensor(out=m2[:, half * 512:(half + 1) * 512], in0=PS[6][:, :],
                                                                       in1=gates[:, 1024 + half * 512:1024 + (half + 1) * 512], op=ALU.mult),
                           reads=["ps6", gn], writes=["m2"])
                    op("pool", lambda v: v.tensor_tensor(out=mb[:], in0=m1[:], in1=m2[:], op=ALU.add), reads=["m1", "m2"], writes=["mb"])
                    psb7 = PS[7][:].bitcast(BF16)
                    for kc in range(8):
                        op("pe", lambda t, kc=kc: t.transpose(psb7[:, kc * 128:(kc + 1) * 128], mb[:, kc * 128:(kc + 1) * 128], ident_b[:]),
                           reads=["mb", "ident_b"], writes=["ps7"])
                    op("act", lambda a: a.activation(out=mT[:].rearrange("p c n -> p (c n)"), in_=psb7[:, :], func=AF.Copy),
                       reads=["ps7"], writes=["mT"])
                    for half in range(2):
                        for kc in range(8):
                            op("pe", lambda t, half=half, kc=kc: t.matmul(PS[5 + half][:, :], mT[:, kc, :], wo[:, kc, half * 512:(half + 1) * 512],
                                                                         start=(kc == 0), stop=(kc == 7)), reads=["mT", "wo"], writes=["ps%d" % (5 + half)])
                        op("dve", lambda v, half=half: v.tensor_tensor(out=m1[:, half * 512:(half + 1) * 512], in0=PS[5 + half][:, :],
                                                                       in1=rep[:, who, 0, half * 512:(half + 1) * 512], op=ALU.mult),
                           reads=["ps%d" % (5 + half), "rep"], writes=["m1"])
                    op("dve", lambda v, xb=xb: v.scalar_tensor_tensor(out=m2[:], in0=xb[:], scalar=ALPHA, in1=m1[:], op0=ALU.mult, op1=ALU.add),
                       reads=[xn, "m1"], writes=["m2"])
                    ln_stats(m2, "m2", lnB)
                    op("act", lambda a: a.activation(out=m1[:], in_=m2[:], func=AF.Identity, bias=lnB["nb"][:], scale=lnB["rstd"][:]),
                       reads=["m2", "nbB", "rstdB"], writes=["m1"])
                    op("pool", lambda v: v.tensor_tensor(out=m1[:], in0=m1[:], in1=lnr[:, 0, :], op=ALU.mult), reads=["m1", "lnr"], writes=["m1"])
                    op("pool", lambda v: v.tensor_tensor(out=m1[:], in0=m1[:], in1=lnr[:, 1, :], op=ALU.add), reads=["m1", "lnr"], writes=["m1"])
                    dma("sp", lambda q, stile=stile: q.dma_start(out=xbufA[stile * 128:(stile + 1) * 128, :], in_=m1[:]),
                        reads=["m1"], writes=["xbufA_%d" % stile])
                    ln_stats(m1, "m1", lnB)
                    op("act", lambda a: a.activation(out=m2[:], in_=m1[:], func=AF.Identity, bias=lnB["nb"][:], scale=lnB["rstd"][:]),
                       reads=["m1", "nbB", "rstdB"], writes=["m2"])
                    op("dve", lambda v: v.tensor_tensor(out=m2[:], in0=m2[:], in1=rep[:, who, 2, :], op=ALU.mult), reads=["m2", "rep"], writes=["m2"])
                    op("dve", lambda v: v.tensor_tensor(out=m2[:], in0=m2[:], in1=rep[:, who, 1, :], op=ALU.add), reads=["m2", "rep"], writes=["m2"])
                    op("pool", lambda v: v.tensor_copy(mb[:], m2[:]), reads=["m2"], writes=["mb"])
                    for half in range(2):
                        for j in range(4):
                            kc = half * 4 + j
                            op("pe", lambda t, kc=kc, j=j, half=half: t.transpose(
                                PS[7 - 2 * half][:, j * 128:(j + 1) * 128], m2[:, kc * 128:(kc + 1) * 128], ident_f[:]),
                                reads=["m2", "ident_f"], writes=["ps%d" % (7 - 2 * half)])
                        op("act", lambda a, half=half: a.activation(out=h2T[:, half * 4:(half + 1) * 4, :].rearrange("p c n -> p (c n)"),
                                                                    in_=PS[7 - 2 * half][:, :], func=AF.Copy),
                           reads=["ps%d" % (7 - 2 * half)], writes=["h2T"])
                    for kc in range(8):
                        op("pe", lambda t, kc=kc: t.matmul(PS[6][:, 0:NE], h2T[:, kc, :], wr_s[:, kc, :], start=(kc == 0), stop=(kc == 7)),
                           reads=["h2T", "wr_s"], writes=["ps6"])
                    op("act", lambda a: a.activation(out=s_t[:], in_=PS[6][:, 0:NE], func=AF.Sigmoid), reads=["ps6"], writes=["s_t"])
                    dma("sp", lambda q, mi=mi: q.dma_start(out=sdram[:, mi, :], in_=s_t[:]), reads=["s_t"], writes=["sdram_%d" % mi])
                    dma("sp", lambda q, mi=mi: q.dma_start(out=hbuf[mi * 128:(mi + 1) * 128, :], in_=mb[:]), reads=["mb"], writes=["hbuf_%d" % mi])
                    S.rec = None
                    allA.append(recA)
                    allB.append(recB)

                for mi_, stile_ in enumerate(q_tiles):
                    tile_body(mi_, stile_)
                S.play(allA[0])
                for mi in range(len(allB)):
                    S.play(allA[mi + 1] if mi + 1 < len(allA) else [], allB[mi])
                S.barrier()

            with ExitStack() as ph:
                n = n_moe
                big = {nm: sb(ph, "rb_" + nm, [128, 36, NE], F32) for nm in ("s", "bsd", "msk", "oh1", "oh2", "tmp", "posf", "tq", "tt")}
                G = {nm: sb(ph, "rg_" + nm, [128, 36 * 8], F32) for nm in ("m1", "n1", "m2", "n2", "t1", "t2", "gs", "goh", "pen")}
                Cc = {nm: sb(ph, "rc_" + nm, [128, 36], F32) for nm in ("gmax", "mx1", "mx2", "w1", "w2", "ws", "d", "p", "v", "cs")}
                A_b = sb(ph, "A_b", [128, 36, NE], BF16)
                destf = sb(ph, "destf", [128, 36, 2], F32)
                hb2 = [sb(ph, "hb%d" % i, [128, D], BF16) for i in range(2)]

                def B(nm):
                    return big[nm][:, 0:n, :]

                def G2(nm):
                    return G[nm][:, 0:n * 8]

                def G3(nm):
                    return G[nm][:, 0:n * 8].rearrange("p (n g) -> p n g", g=8)

                def C(nm):
                    return Cc[nm][:, 0:n]

                def bc(ap2, last):
                    return ap2.unsqueeze(2).to_broadcast([128, ap2.shape[1], last])

                def tt(o, a_, b_, o_, rd, wr):
                    op("dve", lambda v: v.tensor_tensor(out=o, in0=a_, in1=b_, op=o_), reads=rd, writes=wr)

                def red(o, a_, o_, rd, wr):
                    op("dve", lambda v: v.tensor_reduce(out=o, in_=a_, axis=AX.X, op=o_), reads=rd, writes=wr)
                dma("sp", lambda q: q.dma_start(out=B("s"), in_=sdram[:, 0:n, :]), writes=["r_s"])
                tt(B("bsd"), B("s"), rbias_s[:].unsqueeze(1).to_broadcast([128, n, NE]), ALU.add, ["r_s", "rbias_s"], ["r_bsd"])
                bv4 = B("bsd").rearrange("p n (g k) -> p (n g) k", k=4)
                tt(G2("m1"), bv4[:, :, 0], bv4[:, :, 1], ALU.max, ["r_bsd"], ["g_m1"])
                tt(G2("n1"), bv4[:, :, 0], bv4[:, :, 1], ALU.min, ["r_bsd"], ["g_n1"])
                tt(G2("m2"), bv4[:, :, 2], bv4[:, :, 3], ALU.max, ["r_bsd"], ["g_m2"])
                tt(G2("n2"), bv4[:, :, 2], bv4[:, :, 3], ALU.min, ["r_bsd"], ["g_n2"])
                tt(G2("t1"), G2("m1"), G2("m2"), ALU.max, ["g_m1", "g_m2"], ["g_t1"])
                tt(G2("t2"), G2("m1"), G2("m2"), ALU.min, ["g_m1", "g_m2"], ["g_t2"])
                tt(G2("n1"), G2("n1"), G2("n2"), ALU.max, ["g_n1", "g_n2"], ["g_n1"])
                tt(G2("t2"), G2("t2"), G2("n1"), ALU.max, ["g_t2", "g_n1"], ["g_t2"])
                tt(G2("gs"), G2("t1"), G2("t2"), ALU.add, ["g_t1", "g_t2"], ["g_gs"])
                red(C("gmax"), G3("gs"), ALU.max, ["g_gs"], ["c_gmax"])
                tt(G3("goh"), G3("gs"), bc(C("gmax"), 8), ALU.is_equal, ["g_gs", "c_gmax"], ["g_goh"])
                op("dve", lambda v: v.tensor_scalar(G2("pen"), G2("goh"), 8.0, -8.0, op0=ALU.mult, op1=ALU.add), reads=["g_goh"], writes=["g_pen"])
                mv4 = B("msk").rearrange("p n (g k) -> p (n g) k", k=4)
                tt(mv4, bv4, bc(G2("goh"), 4), ALU.mult, ["r_bsd", "g_goh"], ["r_msk"])
                tt(mv4, mv4, bc(G2("pen"), 4), ALU.add, ["r_msk", "g_pen"], ["r_msk"])
                red(C("mx1"), B("msk"), ALU.max, ["r_msk"], ["c_mx1"])
                tt(B("oh1"), B("msk"), bc(C("mx1"), NE), ALU.is_equal, ["r_msk", "c_mx1"], ["r_oh1"])
                op("dve", lambda v: v.scalar_tensor_tensor(out=B("tmp"), in0=B("oh1"), scalar=-16.0, in1=B("msk"), op0=ALU.mult, op1=ALU.add),
                   reads=["r_oh1", "r_msk"], writes=["r_tmp"])
                red(C("mx2"), B("tmp"), ALU.max, ["r_tmp"], ["c_mx2"])
                tt(B("oh2"), B("tmp"), bc(C("mx2"), NE), ALU.is_equal, ["r_tmp", "c_mx2"], ["r_oh2"])
                tt(B("tt"), B("oh1"), B("s"), ALU.mult, ["r_oh1", "r_s"], ["r_tt"])
                red(C("w1"), B("tt"), ALU.add, ["r_tt"], ["c_w1"])
                tt(B("tt"), B("oh2"), B("s"), ALU.mult, ["r_oh2", "r_s"], ["r_tt"])
                red(C("w2"), B("tt"), ALU.add, ["r_tt"], ["c_w2"])
                tt(C("ws"), C("w1"), C("w2"), ALU.add, ["c_w1", "c_w2"], ["c_ws"])
                op("dve", lambda v: v.reciprocal(out=C("ws"), in_=C("ws")), reads=["c_ws"], writes=["c_ws"])
                tt(wts[:, 0:n, 0], C("w1"), C("ws"), ALU.mult, ["c_w1", "c_ws"], ["wts"])
                tt(wts[:, 0:n, 1], C("w2"), C("ws"), ALU.mult, ["c_w2", "c_ws"], ["wts"])
                tt(A_b[:, 0:n, :], B("oh1"), B("oh2"), ALU.add, ["r_oh1", "r_oh2"], ["A_b"])
                X1 = sb(ph, "X1", [128, NE, NE], F32)
                X2 = sb(ph, "X2", [128, NE, NE], F32)
                tri_s = sb(ph, "tri_s", [128, NE, NE], F32)
                sm = {nm: sb(ph, "sm_" + nm, [128, NE], F32) for nm in ("cnt", "rank", "eoffd", "capd", "eos", "base")}
                widx_f = sb(ph, "widx_f", [128, NE, 8], F32)
                dma("sp", lambda q: q.dma_start(out=tri_s[:], in_=tri32), writes=["tri_s"])
                for mi in range(n):
                    op("pe", lambda t, mi=mi: t.matmul(PS[3][:, 0:NE], lt_b[:, 1, :], A_b[:, mi, :], start=(mi == 0), stop=(mi == n - 1)),
                       reads=["lt_b", "A_b"], writes=["ps3"])
                op("act", lambda a: a.activation(out=sm["cnt"][:], in_=PS[3][:, 0:NE], func=AF.Copy), reads=["ps3"], writes=["sm_cnt"])
                c_row = sm["cnt"][:].unsqueeze(2).to_broadcast([128, NE, NE])
                c_col = sm["cnt"][:].unsqueeze(1).to_broadcast([128, NE, NE])
                tt(X1[:], c_col, c_row, ALU.is_gt, ["sm_cnt"], ["X1"])
                tt(X2[:], c_col, c_row, ALU.is_equal, ["sm_cnt"], ["X2"])
                tt(X2[:], X2[:], tri_s[:], ALU.mult, ["X2", "tri_s"], ["X2"])
                tt(X1[:], X1[:], X2[:], ALU.add, ["X1", "X2"], ["X1"])
                red(sm["rank"][:], X1[:], ALU.add, ["X1"], ["sm_rank"])
                io_col = eoff_s[:].unsqueeze(1).to_broadcast([128, NE, NE])
                io_row = eoff_s[:].unsqueeze(2).to_broadcast([128, NE, NE])
                tt(X1[:], sm["rank"][:].unsqueeze(2).to_broadcast([128, NE, NE]), io_col, ALU.is_equal, ["sm_rank", "eoff_s"], ["X1"])
                tt(X2[:], X1[:], slot_s[:, 0, :].unsqueeze(1).to_broadcast([128, NE, NE]), ALU.mult, ["X1", "slot_s"], ["X2"])
                red(sm["eoffd"][:], X2[:], ALU.add, ["X2"], ["sm_eoffd"])
                tt(X2[:], X1[:], slot_s[:, 1, :].unsqueeze(1).to_broadcast([128, NE, NE]), ALU.mult, ["X1", "slot_s"], ["X2"])
                red(sm["capd"][:], X2[:], ALU.add, ["X2"], ["sm_capd"])
                tt(X2[:], X1[:], io_row, ALU.mult, ["X1", "eoff_s"], ["X2"])
                red(sm["eos"][:], X2[:].rearrange("p e s -> p s e"), ALU.add, ["X2"], ["sm_eos"])
                op("dve", lambda v: v.tensor_scalar(sm["base"][:], sm["eos"][:], 128.0, pk_s[:, 0:1], op0=ALU.mult, op1=ALU.add),
                   reads=["sm_eos", "pk_s"], writes=["sm_base"])
                op("dve", lambda v: v.tensor_scalar(sm["base"][:], sm["base"][:], float(layer * NE * 128), None, op0=ALU.add),
                   reads=["sm_base"], writes=["sm_base"])
                tt(widx_f[:], sm["base"][:].unsqueeze(2).to_broadcast([128, NE, 8]), pk_s[:, 1:9].unsqueeze(1).to_broadcast([128, NE, 8]),
                   ALU.add, ["sm_base", "pk_s"], ["widx_f"])
                op("dve", lambda v: v.tensor_copy(widx[:], widx_f[:]), reads=["widx_f"], writes=["widx"])
                for mi in range(n):
                    bk = mi // 16
                    reg = PS[bk][:, (mi % 16) * NE:(mi % 16 + 1) * NE]
                    op("pe", lambda t, reg=reg, mi=mi: t.matmul(reg, lt_b[:, 0, :], A_b[:, mi, :], start=True, stop=(mi == 0)),
                       reads=["lt_b", "A_b"], writes=["ps%d" % bk])
                    for j in range(mi):
                        op("pe", lambda t, reg=reg, j=j, mi=mi: t.matmul(reg, lt_b[:, 1, :], A_b[:, j, :], start=False, stop=(j == mi - 1)),
                           reads=["lt_b", "A_b"], writes=["ps%d" % bk])
                for bk in range((n + 15) // 16):
                    t0_, t1_ = bk * 16, min(n, bk * 16 + 16)
                    op("act", lambda a, bk=bk, t0_=t0_, t1_=t1_: a.activation(
                        out=big["posf"][:, t0_:t1_, :], in_=PS[bk][:, 0:(t1_ - t0_) * NE].rearrange("p (n e) -> p n e", e=NE), func=AF.Copy),
                        reads=["ps%d" % bk], writes=["r_posf"])
                tt(B("tq"), B("posf"), sm["eoffd"][:].unsqueeze(1).to_broadcast([128, n, NE]), ALU.add, ["r_posf", "sm_eoffd"], ["r_tq"])
                for k, oh in enumerate(("oh1", "oh2")):
                    tt(B("tt"), B(oh), B("tq"), ALU.mult, ["r_" + oh, "r_tq"], ["r_tt"])
                    red(C("d"), B("tt"), ALU.add, ["r_tt"], ["c_d"])
                    tt(B("tt"), B(oh), B("posf"), ALU.mult, ["r_" + oh, "r_posf"], ["r_tt"])
                    red(C("p"), B("tt"), ALU.add, ["r_tt"], ["c_p"])
                    tt(B("tt"), B(oh), sm["capd"][:].unsqueeze(1).to_broadcast([128, n, NE]), ALU.mult, ["r_" + oh, "sm_capd"], ["r_tt"])
                    red(C("cs"), B("tt"), ALU.add, ["r_tt"], ["c_cs"])
                    tt(C("v"), C("p"), C("cs"), ALU.is_lt, ["c_p", "c_cs"], ["c_v"])
                    op("dve", lambda v: v.tensor_scalar(C("d"), C("d"), dumpp_s[:, 0:1], None, op0=ALU.subtract), reads=["c_d", "dumpp_s"], writes=["c_d"])
                    tt(C("d"), C("d"), C("v"), ALU.mult, ["c_d", "c_v"], ["c_d"])
                    op("dve", lambda v, k=k: v.tensor_scalar(destf[:, 0:n, k], C("d"), dumpp_s[:, 0:1], None, op0=ALU.add),
                       reads=["c_d", "dumpp_s"], writes=["destf"])
                op("dve", lambda v: v.tensor_copy(dest_i[:, 0:n, :], destf[:, 0:n, :]), reads=["destf"], writes=["dest_i"])
                for mi in range(n):
                    hb = hb2[mi % 2]
                    hn = "hb%d" % (mi % 2)
                    dma("sp", lambda q, hb=hb, mi=mi: q.dma_start(out=hb[:], in_=hbuf[mi * 128:(mi + 1) * 128, :]), writes=[hn])
                    for k in range(2):
                        dma("pool", lambda q, mi=mi, k=k, hb=hb: q.indirect_dma_start(
                            out=xg, out_offset=bass.IndirectOffsetOnAxis(ap=dest_i[:, mi, k:k + 1].bitcast(U32), axis=0),
                            in_=hb[:], in_offset=None), reads=[hn, "dest_i"], writes=["xg_%d_%d" % (mi, k)])
                if stop == "p2":
                    dump("xbufA", xbufA, [NST * 128, D])
                    dump("dest", dest_i[:].rearrange("p a b -> p (a b)"), [128, 72], I32)
                    dump("wts", wts[:].rearrange("p a b -> p (a b)"), [128, 72])
                    dump("xg", xg, [NSLOT + 128, D], BF16)
                finish("p2")
                S.barrier()

            with ExitStack() as ph:
                wg = [sb(ph, "wg%d" % i, [128, 8, D], BF16) for i in range(2)]
                wu = [sb(ph, "wu%d" % i, [128, 8, D], BF16) for i in range(2)]
                wd = [sb(ph, "wd%d" % i, [128, 8, D], BF16) for i in range(2)]
                xgt = [sb(ph, "xgt%d" % i, [128, D], BF16) for i in range(2)]
                HC = 384
                XTs = [sb(ph, "XT%d" % i, [128, 8, HC], BF16) for i in range(2)]
                sgs = [sb(ph, "sg%d" % i, [128, HC], BF16) for i in range(2)]
                aTs = [sb(ph, "aT%d" % i, [128, 8, HC], BF16) for i in range(2)]
                yo = [sb(ph, "yo%d" % i, [128, D], F32) for i in range(3)]
                cnt = {"ld": 0, "yi": 0, "sg": 0}
                halves = []
                order = []
                for i_ in range(NE // 2):
                    order += [i_, NE - 1 - i_]
                for pos_, sl in enumerate(order):
                    c_, r_ = SLOT_CAPS[sl], SLOT_OFF[sl]
                    while c_ > 0:
                        w_ = 384 if (c_ >= 384 and c_ != 512) else 256
                        halves.append((pos_, r_, w_))
                        r_ += w_
                        c_ -= w_
                first_of = {}
                for hi_, (sl, r_, w_) in enumerate(halves):
                    first_of.setdefault(sl, hi_)
                wflat = {"wg": w_eg, "wu": w_eu, "wd": w_ed}

                def emit_W(e):
                    par = e % 2
                    for (wbuf, nm) in ((wg[par], "wg"), (wu[par], "wu"), (wd[par], "wd")):
                        dma("pool", lambda q, wbuf=wbuf, nm=nm, e=e: q.indirect_dma_start(
                            out=wbuf[:].rearrange("p k n -> p (k n)"), out_offset=None, in_=wflat[nm],
                            in_offset=bass.IndirectOffsetOnAxis(ap=widx[:, order[e], 0:1].bitcast(U32), axis=0)),
                            reads=["widx"], writes=["%s%d" % (nm, par)])

                def emit_T(hidx):
                    e, row0, ncol = halves[hidx]
                    XT = XTs[hidx % 2]
                    for r in range(ncol // 128):
                        ld = cnt["ld"]
                        cnt["ld"] += 1
                        xb = xgt[ld % 2]
                        xn = "xgt%d" % (ld % 2)
                        tb = 6 + (ld % 2)
                        dma("sp", lambda q, xb=xb, row0=row0, r=r: q.dma_start(out=xb[:], in_=xg[row0 + r * 128:row0 + (r + 1) * 128, :]),
                            writes=[xn])
                        psb = PS[tb][:].bitcast(BF16)
                        for kc in range(8):
                            op("pe", lambda t, kc=kc, xb=xb, psb=psb: t.transpose(psb[:, kc * 128:(kc + 1) * 128], xb[:, kc * 128:(kc + 1) * 128], ident_b[:]),
                               reads=[xn, "ident_b"], writes=["ps%d" % tb])
                        op("act", lambda a, r=r, psb=psb, XT=XT: a.activation(out=XT[:, :, r * 128:(r + 1) * 128],
                                                                             in_=psb[:, :].rearrange("p (c n) -> p c n", n=128), func=AF.Copy),
                           reads=["ps%d" % tb], writes=["XT%d" % (hidx % 2)])

                def emit_GU(hidx):
                    e, row0, ncol = halves[hidx]
                    par = e % 2
                    XT, aT = XTs[hidx % 2], aTs[hidx % 2]
                    xtn, atn = "XT%d" % (hidx % 2), "aT%d" % (hidx % 2)
                    for dc in range(8):
                        gb = (dc % 2) * 2
                        sgi = cnt["sg"] % 2
                        cnt["sg"] += 1
                        sg = sgs[sgi]
                        for kc in range(8):
                            op("pe", lambda t, dc=dc, kc=kc, gb=gb: t.matmul(PS[gb][:, 0:ncol], wg[par][:, kc, dc * 128:(dc + 1) * 128], XT[:, kc, 0:ncol],
                                                                            start=(kc == 0), stop=(kc == 7)), reads=["wg%d" % par, xtn], writes=["ps%d" % gb])
                        for kc in range(8):
                            op("pe", lambda t, dc=dc, kc=kc, gb=gb: t.matmul(PS[gb + 1][:, 0:ncol], wu[par][:, kc, dc * 128:(dc + 1) * 128], XT[:, kc, 0:ncol],
                                                                            start=(kc == 0), stop=(kc == 7)), reads=["wu%d" % par, xtn], writes=["ps%d" % (gb + 1)])
                        op("act", lambda a, gb=gb, sg=sg: a.activation(out=sg[:, 0:ncol], in_=PS[gb][:, 0:ncol], func=AF.Silu), reads=["ps%d" % gb], writes=["sg%d" % sgi])
                        op("dve", lambda v, gb=gb, dc=dc, sg=sg: v.tensor_tensor(out=aT[:, dc, 0:ncol], in0=PS[gb + 1][:, 0:ncol], in1=sg[:, 0:ncol], op=ALU.mult),
                           reads=["ps%d" % (gb + 1), "sg%d" % sgi], writes=[atn])

                def emit_D(hidx):
                    e, row0, ncol = halves[hidx]
                    par = e % 2
                    aT = aTs[hidx % 2]
                    atn = "aT%d" % (hidx % 2)
                    for r in range(ncol // 128):
                        yi = cnt["yi"]
                        cnt["yi"] += 1
                        yb = yo[yi % 3]
                        yn = "yo%d" % (yi % 3)
                        for half in range(2):
                            bank = 4 + half
                            for kc in range(8):
                                op("pe", lambda t, r=r, half=half, kc=kc, bank=bank: t.matmul(
                                    PS[bank][:, :], aT[:, kc, r * 128:(r + 1) * 128], wd[par][:, kc, half * 512:(half + 1) * 512],
                                    start=(kc == 0), stop=(kc == 7)), reads=[atn, "wd%d" % par], writes=["ps%d" % bank])
                        op("act", lambda a, yb=yb: a.activation(out=yb[:, 0:512], in_=PS[4][:, :], func=AF.Copy), reads=["ps4"], writes=[yn])
                        op("dve", lambda v, yb=yb: v.tensor_copy(yb[:, 512:1024], PS[5][:, :]), reads=["ps5"], writes=[yn])
                        dma("sp", lambda q, yb=yb, row0=row0, r=r: q.dma_start(out=yg[row0 + r * 128:row0 + (r + 1) * 128, :], in_=yb[:]),
                            reads=[yn], writes=["yg_%d" % (row0 + r * 128)])

                emit_W(0)
                emit_T(0)
                for hidx, (e, row0_, ncol_) in enumerate(halves):
                    if first_of[e] == hidx and e + 1 < NE:
                        emit_W(e + 1)
                    emit_GU(hidx)
                    if hidx + 1 < len(halves):
                        emit_T(hidx + 1)
                    emit_D(hidx)
                if stop == "moe":
                    dump("yg", yg[0:3072, :], [3072, D])
                finish("moe")
                S.barrier()

            with ExitStack() as ph:
                y1 = [sb(ph, "y1_%d" % i, [128, D], F32) for i in range(2)]
                y2 = [sb(ph, "y2_%d" % i, [128, D], F32) for i in range(2)]
                xc = [sb(ph, "xc%d" % i, [128, D], F32) for i in range(2)]
                f1s = [sb(ph, "f1_%d" % i, [128, D], F32) for i in range(2)]
                zs = [sb(ph, "z_%d" % i, [128, D], F32) for i in range(2)]
                xo = [sb(ph, "xo%d" % i, [128, D], F32) for i in range(2)]
                st6s = [sb(ph, "st6c%d" % i, [128, 2, 6], F32) for i in range(2)]
                mvs = [sb(ph, "mvc%d" % i, [128, 2], F32) for i in range(2)]
                rstds = [sb(ph, "rstdc%d" % i, [128, 1], F32) for i in range(2)]
                nbs = [sb(ph, "nbc%d" % i, [128, 1], F32) for i in range(2)]
                lnr = sb(ph, "lnr2", [128, 2, D], F32)
                g2r = sb(ph, "g2r", [128, 2, D], F32)
                dma("sp", lambda q: q.dma_start(out=g2r[:], in_=g2buf[layer]), writes=["g2r"])
                dma("sp", lambda q: q.dma_start(out=lnr[:], in_=ln_rep[layer][2:4].rearrange("a p n -> p a n")), writes=["lnr"])
                for mi, stile in enumerate(q_tiles):
                    p = mi % 2
                    f1, z, st6, mv, rstd, nb = f1s[p], zs[p], st6s[p], mvs[p], rstds[p], nbs[p]
                    F1, Z, ST, MV, RS, NB = "f1_%d" % p, "z_%d" % p, "st6c%d" % p, "mvc%d" % p, "rstdc%d" % p, "nbc%d" % p
                    who = 1 if stile >= NT else 0
                    dma("pool", lambda q, mi=mi, p=p: q.indirect_dma_start(
                        out=y1[p][:], out_offset=None, in_=yg,
                        in_offset=bass.IndirectOffsetOnAxis(ap=dest_i[:, mi, 0:1].bitcast(U32), axis=0)),
                        reads=["yg", "dest_i"], writes=["y1_%d" % p])
                    dma("pool", lambda q, mi=mi, p=p: q.indirect_dma_start(
                        out=y2[p][:], out_offset=None, in_=yg,
                        in_offset=bass.IndirectOffsetOnAxis(ap=dest_i[:, mi, 1:2].bitcast(U32), axis=0)),
                        reads=["yg", "dest_i"], writes=["y2_%d" % p])
                    dma("sp", lambda q, stile=stile, p=p: q.dma_start(out=xc[p][:], in_=xbufA[stile * 128:(stile + 1) * 128, :]),
                        writes=["xc%d" % p])
                    op("dve", lambda v, mi=mi, p=p: v.tensor_scalar(f1[:], y1[p][:], wts[:, mi, 0:1], None, op0=ALU.mult),
                       reads=["y1_%d" % p, "wts"], writes=[F1])
                    op("dve", lambda v, mi=mi, p=p: v.scalar_tensor_tensor(out=f1[:], in0=y2[p][:], scalar=wts[:, mi, 1:2], in1=f1[:],
                                                                            op0=ALU.mult, op1=ALU.add), reads=["y2_%d" % p, "wts", F1], writes=[F1])
                    op("pool", lambda v: v.tensor_tensor(out=f1[:], in0=f1[:], in1=g2r[:, who, :], op=ALU.mult), reads=[F1, "g2r"], writes=[F1])
                    op("dve", lambda v, p=p: v.scalar_tensor_tensor(out=z[:], in0=xc[p][:], scalar=ALPHA, in1=f1[:], op0=ALU.mult, op1=ALU.add),
                       reads=["xc%d" % p, F1], writes=[Z])
                    for hh in range(2):
                        op("dve", lambda v, hh=hh: v.bn_stats(out=st6[:, hh, :], in_=z[:, hh * 512:(hh + 1) * 512]), reads=[Z], writes=[ST])
                    op("dve", lambda v: v.bn_aggr(out=mv[:], in_=st6[:].rearrange("p a b -> p (a b)")), reads=[ST], writes=[MV])
                    op("act", lambda a: a.activation(out=rstd[:], in_=mv[:, 1:2], func=AF.Sqrt, bias=epsc[:], scale=1.0),
                       reads=[MV, "epsc"], writes=[RS])
                    op("dve", lambda v: v.reciprocal(out=rstd[:], in_=rstd[:]), reads=[RS], writes=[RS])
                    op("dve", lambda v: v.scalar_tensor_tensor(out=nb[:], in0=mv[:, 0:1], scalar=-1.0, in1=rstd[:], op0=ALU.mult, op1=ALU.mult),
                       reads=[MV, RS], writes=[NB])
                    op("act", lambda a: a.activation(out=z[:], in_=z[:], func=AF.Identity, bias=nb[:], scale=rstd[:]),
                       reads=[Z, NB, RS], writes=[Z])
                    op("pool", lambda v: v.tensor_tensor(out=z[:], in0=z[:], in1=lnr[:, 0, :], op=ALU.mult), reads=[Z, "lnr"], writes=[Z])
                    op("dve", lambda v, p=p: v.tensor_tensor(out=xo[p][:], in0=z[:], in1=lnr[:, 1, :], op=ALU.add), reads=[Z, "lnr"], writes=["xo%d" % p])
                    if not last:
                        dma("sp", lambda q, stile=stile, p=p: q.dma_start(out=xbufB[stile * 128:(stile + 1) * 128, :], in_=xo[p][:]),
                            reads=["xo%d" % p], writes=["xbufB_%d" % stile])
                    else:
                        dma("sp", lambda q, stile=stile, p=p: q.dma_start(out=out[(stile - 2) * 128:(stile - 1) * 128, :], in_=xo[p][:]),
                            reads=["xo%d" % p], writes=["out_%d" % stile])
                if stop == "l0":
                    dump("xbufB", xbufB, [NST * 128, D])
                finish("l0")
                S.barrier()
    except _Stop:
        return nc
    S.barrier()
    top.close()
    return nc


def _host_inputs(x, c, ctx, c_ctx, w_ada, b_ada, w_in, w_pool_grp, pool_scale, w_pool_br, w_attn_br,
                 attn_sink, w_o, ln1_g, ln1_b, w_router, router_bias, w_exp_gate, w_exp_up, w_exp_down,
                 ln2_g, ln2_b):
    f32 = np.float32
    phys = [h for i in range(4) for h in (i, i + 4)]
    qcols = np.concatenate([np.arange(512 + h * 64, 512 + (h + 1) * 64) for h in phys])
    cols = np.concatenate([np.arange(0, 512), qcols, np.arange(1024, 1280), np.arange(1280, 3328)])
    w_in_p = np.ascontiguousarray(w_in[:, :, cols])
    arows = np.concatenate([np.arange(h * 64, (h + 1) * 64) for h in phys])
    w_abr_p = np.ascontiguousarray(w_attn_br[:, arows, :])
    sink_rep = np.ascontiguousarray(np.broadcast_to(attn_sink[:, None, phys], (DEPTH, 128, 8))).astype(f32)
    bada_fm = np.ascontiguousarray(b_ada[:, :2048].reshape(DEPTH, 16, 128).transpose(0, 2, 1)).astype(f32)
    bada_rep = np.ascontiguousarray(np.broadcast_to(b_ada[:, None, 2048:], (DEPTH, 128, 4 * D))).astype(f32)
    pscale = np.ascontiguousarray(pool_scale.reshape(DEPTH, 4, 128).transpose(0, 2, 1)).astype(f32)
    ln_rep = np.ascontiguousarray(np.broadcast_to(np.stack([ln1_g, ln1_b, ln2_g, ln2_b], 1)[:, :, None, :], (DEPTH, 4, 128, D))).astype(f32)
    rbias_rep = np.ascontiguousarray(np.broadcast_to(router_bias[None, :], (128, NE))).astype(f32)
    rot = np.zeros((128, 128), f32)
    for dp in range(128):
        if dp % 32 < 16:
            rot[dp + 16, dp] = -1.0
        else:
            rot[dp - 16, dp] = 1.0
    ident = np.eye(128, dtype=f32)
    kl = np.arange(128)[:, None]
    ql = np.arange(128)[None, :]
    mtri = np.stack([(kl >= ql), (kl <= ql)]).astype(f32)
    ltm = np.stack([(kl < ql), np.ones((128, 128), bool)]).astype(f32)
    eoff = np.ascontiguousarray(np.broadcast_to(np.arange(NE)[None, :], (128, NE))).astype(f32)
    slottab = np.ascontiguousarray(np.broadcast_to(np.stack([np.array(SLOT_OFF), np.array(SLOT_CAPS)], 0)[None], (128, 2, NE))).astype(f32)
    ee = np.arange(NE)
    tri32 = np.ascontiguousarray(np.broadcast_to((ee[None, :] < ee[:, None])[None], (128, NE, NE))).astype(f32)
    pk = np.concatenate([np.arange(128)[:, None], np.zeros((128, 8))], 1).astype(f32)

    def relay(w):
        return np.ascontiguousarray(w.reshape(DEPTH, NE, 8, 128, D).transpose(0, 1, 3, 2, 4)).reshape(DEPTH * NE * 128, 8 * D)
    w_eg_r, w_eu_r, w_ed_r = relay(w_exp_gate), relay(w_exp_up), relay(w_exp_down)
    dumpp = (NSLOT + np.arange(128))[:, None].astype(f32)
    inv_freq = (10000.0 ** (-np.arange(0, 32, 2, dtype=np.float32) / 32)).astype(np.float32)

    def pool_tab(t_abs, L):
        tab = np.zeros((4, len(t_abs)), f32)
        for g, w in enumerate((2, 4, 8, 16)):
            lo = np.clip(t_abs - w // 2, 0, L - 1)
            hi = np.clip(t_abs - w // 2 + w - 1, 0, L - 1)
            tab[g] = 1.0 / (hi - lo + 1)
        return tab

    in_maps = []
    for core in range(NCORE):
        b, j = core // 4, core % 4
        s = j * OWN
        t_abs = np.arange(s - HALO, s + OWN + HALO)
        valid = (t_abs >= 0) & (t_abs < L_SEQ)
        xe = np.zeros((NTOK, D), f32)
        xe[valid] = x[b, t_abs[valid]]
        tc = np.clip(t_abs, 0, L_SEQ - 1)
        row = (tc // 64).astype(np.float32)
        col = (tc % 64).astype(np.float32)
        ang = np.zeros((64, NTOK), np.float32)
        ang_r = row[None, :] * inv_freq[:, None]
        ang_c = col[None, :] * inv_freq[:, None]
        ang[0:16], ang[16:32], ang[32:48], ang[48:64] = ang_r, ang_r, ang_c, ang_c
        ropeC = np.concatenate([np.cos(ang), np.cos(ang)], 0).astype(f32)
        ropeS = np.concatenate([np.sin(ang), np.sin(ang)], 0).astype(f32)
        vmask = np.ones((128, NT + 2), f32)
        vmask[:, :NT] = valid.reshape(NT, 128)[:, 0][None, :].astype(f32)
        pinv = np.zeros((4, 128, 4, 128), f32)
        pinv[0] = pool_tab(np.clip(t_abs[256:384], 0, L_SEQ - 1), L_SEQ)[None]
        pinv[1] = pool_tab(np.clip(t_abs[(NT - 3) * 128:(NT - 2) * 128], 0, L_SEQ - 1), L_SEQ)[None]
        pinv[2] = pool_tab(np.arange(0, 128), CTX)[None]
        pinv[3] = pool_tab(np.arange(128, 256), CTX)[None]
        cc = np.stack([c[b], c_ctx], 0)
        cT2 = np.ascontiguousarray(cc.reshape(2, 8, 128).transpose(2, 1, 0)).astype(f32)
        cTrep = np.ascontiguousarray(np.broadcast_to(cc.reshape(2, 8, 128).transpose(0, 2, 1)[:, :, :, None], (2, 128, 8, 128))).astype(f32)
        in_maps.append({
            "xe": xe, "ctxin": np.ascontiguousarray(ctx[b]), "cT2": cT2, "cTrep": cTrep, "w_ada": w_ada,
            "bada_fm": bada_fm, "bada_rep": bada_rep, "w_in": w_in_p, "w_pg": w_pool_grp, "pscale": pscale,
            "w_pbr": w_pool_br, "w_abr": w_abr_p, "sink_rep": sink_rep, "w_o": w_o, "ln_rep": ln_rep,
            "w_router": w_router, "rbias_rep": rbias_rep, "w_eg": w_eg_r, "w_eu": w_eu_r, "w_ed": w_ed_r,
            "ropeC": ropeC, "ropeS": ropeS, "rotm": rot, "ident": ident, "mtri": mtri, "ltm": ltm, "vmask": vmask,
            "pinv": pinv, "eoff": eoff, "dumpp": dumpp, "slottab": slottab, "tri32": tri32, "pk": pk,
        })
    return in_maps


_NC = None


def kernel(**inputs):
    global _NC
    inputs = {k: np.asarray(v) for k, v in inputs.items()}
    in_maps = _host_inputs(**inputs)
    if _NC is None:
        _NC = build_program()
    res = run_bass_kernel_spmd(_NC, in_maps, core_ids=list(range(NCORE)))
    outs = [np.asarray(r["out"]) for r in res.results]
    full = np.stack([np.concatenate(outs[0:4], 0), np.concatenate(outs[4:8], 0)], 0)
    return full.astype(np.float32)
```
